# Optimizing a Trainium2 kernel written in Bass

```python
import jax, jax.numpy as jnp
from jax import lax
import numpy as np

D_MODEL = 2048
BATCH = 8
SEQ = 2048
DEPTH = 1

CTX_LEN = 256
GRID_W = 64
D_MIX = D_MODEL
NA_HEADS = 8
HEAD_DIM = 128
D_ATTN = NA_HEADS * HEAD_DIM
D_CONV = D_MIX - D_ATTN
CONV_GROUPS = 8
CONV_W = 3
WIN_H = 8
WIN_W = 16
PEER_HEADS = 8
PEER_TOPK = 16
N_KEYS = 128
N_EXPERTS = N_KEYS * N_KEYS
PEER_QDIM = 256
PEER_BLOCK = 128
N_MOD = 6
EPS = 1e-6
NEG_INF = -1e30

kernel_name = "hymba_natten_shortconv_peer_dit"


def rms_norm(x, g):
    xf = x.astype(jnp.float32)
    y = xf * lax.rsqrt(jnp.mean(xf * xf, axis=-1, keepdims=True) + EPS)
    return (y * g.astype(jnp.float32)).astype(x.dtype)


def modulate(h, shift, scale):
    return h * (1 + scale) + shift


def split_proj(p):
    return jnp.split(p, [D_ATTN, 2 * D_ATTN, 3 * D_ATTN, 3 * D_ATTN + D_CONV, 3 * D_ATTN + 2 * D_CONV], axis=-1)


def to_heads(t):
    b, n, _ = t.shape
    return t.reshape(b, n, NA_HEADS, HEAD_DIM).transpose(0, 2, 1, 3)


def from_heads(t):
    b, h, n, d = t.shape
    return t.transpose(0, 2, 1, 3).reshape(b, n, h * d)


def group_rms(y, g):
    gs, dg = g.shape
    return rms_norm(y.reshape(*y.shape[:-1], gs, dg), g).reshape(y.shape)


def short_conv(u, b_gate, c_gate, w_conv):
    z = c_gate * u
    y = lax.conv_general_dilated(z, w_conv[:, None, :], window_strides=(1,),
                                 padding=((CONV_W // 2, CONV_W // 2),),
                                 dimension_numbers=('NWC', 'WIO', 'NWC'),
                                 feature_group_count=D_CONV)
    return b_gate * y


def neighbourhood_attention(q, k, v, k_ctx, v_ctx, rpb):
    b, h, n, dh = q.shape
    rows = n // GRID_W
    kh = min(WIN_H, rows)
    scale = dh ** -0.5
    qg = q.reshape(b, h, rows, GRID_W, dh)
    kg = k.reshape(b, h, rows, GRID_W, dh)
    vg = v.reshape(b, h, rows, GRID_W, dh)
    col = jnp.arange(GRID_W)
    col_start = jnp.clip(col - WIN_W // 2, 0, GRID_W - WIN_W)
    col_in = (col[None, :] >= col_start[:, None]) & (col[None, :] < col_start[:, None] + WIN_W)
    dc = jnp.clip(col[None, :] - col[:, None], -(WIN_W - 1), WIN_W - 1) + (WIN_W - 1)
    rpb32 = rpb.astype(jnp.float32)

    def row_block(r):
        rs = jnp.clip(r - kh // 2, 0, rows - kh)
        q_blk = lax.dynamic_index_in_dim(qg, r, axis=2, keepdims=False)
        k_blk = lax.dynamic_slice_in_dim(kg, rs, kh, axis=2)
        v_blk = lax.dynamic_slice_in_dim(vg, rs, kh, axis=2)
        s_win = jnp.einsum('bhqd,bhikd->bhqik', q_blk, k_blk).astype(jnp.float32) * scale
        dr = rs + jnp.arange(kh) - r + (WIN_H - 1)
        bias = rpb32[:, dr[None, :, None], dc[:, None, :]]
        s_win = jnp.where(col_in[:, None, :], s_win + bias, NEG_INF)
        s_ctx = jnp.einsum('bhqd,bhld->bhql', q_blk, k_ctx).astype(jnp.float32) * scale
        p = jax.nn.softmax(jnp.concatenate([s_win.reshape(b, h, GRID_W, kh * GRID_W), s_ctx], axis=-1), axis=-1)
        p = p.astype(v.dtype)
        o = jnp.einsum('bhqn,bhnd->bhqd', p[..., :kh * GRID_W], v_blk.reshape(b, h, kh * GRID_W, dh))
        return o + jnp.einsum('bhql,bhld->bhqd', p[..., kh * GRID_W:], v_ctx)

    out = lax.map(row_block, jnp.arange(rows))
    return out.transpose(1, 0, 3, 2, 4).reshape(b, n, h * dh)


def context_attention(q, k, v):
    s = jnp.einsum('bhqd,bhkd->bhqk', q, k).astype(jnp.float32) * (q.shape[-1] ** -0.5)
    p = jax.nn.softmax(s, axis=-1).astype(v.dtype)
    return from_heads(jnp.einsum('bhqk,bhkd->bhqd', p, v))


def peer(h, w_pq, sub_keys, u_tab, v_tab):
    b, n, d = h.shape
    tok = h.reshape(-1, PEER_BLOCK, d)

    def block(hb):
        nb = hb.shape[0]
        q = (hb @ w_pq).reshape(nb, PEER_HEADS, 2, PEER_QDIM // 2)
        s = jnp.einsum('nhpd,hpkd->nhpk', q, sub_keys).astype(jnp.float32)
        s_top, i_top = lax.top_k(s, PEER_TOPK)
        cand = s_top[:, :, 0, :, None] + s_top[:, :, 1, None, :]
        cand_idx = i_top[:, :, 0, :, None] * N_KEYS + i_top[:, :, 1, None, :]
        best_s, best_pos = lax.top_k(cand.reshape(nb, PEER_HEADS, PEER_TOPK * PEER_TOPK), PEER_TOPK)
        expert = jnp.take_along_axis(cand_idx.reshape(nb, PEER_HEADS, PEER_TOPK * PEER_TOPK), best_pos, axis=-1)
        gate = jax.nn.softmax(best_s, axis=-1).astype(hb.dtype).reshape(nb, PEER_HEADS * PEER_TOPK)
        expert = expert.reshape(nb, PEER_HEADS * PEER_TOPK)
        u = u_tab[expert]
        v = v_tab[expert]
        act = jax.nn.gelu(jnp.einsum('nd,ned->ne', hb, u), approximate=False) * gate
        return jnp.einsum('ne,ned->nd', act, v)

    return lax.map(block, tok).reshape(b, n, d)


def setup_inputs(seed: int = 0) -> dict:
    key = jax.random.key(seed)
    ks = jax.random.split(key, 20)
    f32 = jnp.float32
    nrm = lambda k, shape, s: jax.random.normal(k, shape, f32) * s
    gain = lambda k, shape: 1.0 + 0.05 * jax.random.normal(k, shape, f32)
    L = DEPTH
    return {
        "x": nrm(ks[0], (BATCH, SEQ, D_MODEL), 1.0),
        "c": nrm(ks[1], (BATCH, D_MODEL), 1.0),
        "ctx": nrm(ks[2], (BATCH, CTX_LEN, D_MODEL), 1.0),
        "c_ctx": nrm(ks[3], (D_MODEL,), 1.0),
        "w_ada": nrm(ks[4], (L, D_MODEL, N_MOD * D_MODEL), D_MODEL ** -0.5),
        "b_ada": nrm(ks[5], (L, N_MOD * D_MODEL), 0.02),
        "g_norm1": gain(ks[6], (L, D_MODEL)),
        "w_in": nrm(ks[7], (L, D_MODEL, 3 * D_ATTN + 3 * D_CONV), D_MODEL ** -0.5),
        "q_norm": gain(ks[8], (L, HEAD_DIM)),
        "k_norm": gain(ks[9], (L, HEAD_DIM)),
        "rpb": nrm(ks[10], (L, NA_HEADS, 2 * WIN_H - 1, 2 * WIN_W - 1), 0.1),
        "w_conv": nrm(ks[11], (L, CONV_W, D_CONV), CONV_W ** -0.5),
        "g_attn_out": gain(ks[12], (L, NA_HEADS, HEAD_DIM)),
        "g_conv_out": gain(ks[13], (L, CONV_GROUPS, D_CONV // CONV_GROUPS)),
        "w_out": nrm(ks[14], (L, D_MIX, D_MODEL), D_MIX ** -0.5),
        "g_norm2": gain(ks[15], (L, D_MODEL)),
        "w_pq": nrm(ks[16], (L, D_MODEL, PEER_HEADS * PEER_QDIM), D_MODEL ** -0.5),
        "sub_keys": nrm(ks[17], (L, PEER_HEADS, 2, N_KEYS, PEER_QDIM // 2), (PEER_QDIM // 2) ** -0.5),
        "u_experts": nrm(ks[18], (L, N_EXPERTS, D_MODEL), D_MODEL ** -0.5),
        "v_experts": nrm(ks[19], (L, N_EXPERTS, D_MODEL), PEER_HEADS ** -0.5),
    }


def reference(x, c, ctx, c_ctx, w_ada, b_ada, g_norm1, w_in, q_norm, k_norm, rpb, w_conv,
              g_attn_out, g_conv_out, w_out, g_norm2, w_pq, sub_keys, u_experts, v_experts):
    cond = jax.nn.silu(c)
    cond_ctx = jax.nn.silu(c_ctx)
    for l in range(DEPTH):
        update_ctx = l < DEPTH - 1
        mod = (cond @ w_ada[l] + b_ada[l])[:, None, :]
        mod_c = (cond_ctx @ w_ada[l] + b_ada[l])[None, None, :]
        sh1, sc1, gt1, sh2, sc2, gt2 = jnp.split(mod, N_MOD, axis=-1)
        sh1c, sc1c, gt1c, sh2c, sc2c, gt2c = jnp.split(mod_c, N_MOD, axis=-1)

        h = modulate(rms_norm(x, g_norm1[l]), sh1, sc1)
        q, k, v, bg, cg, u = split_proj(h @ w_in[l])
        q = rms_norm(to_heads(q), q_norm[l])
        k = rms_norm(to_heads(k), k_norm[l])
        v = to_heads(v)

        hc = modulate(rms_norm(ctx, g_norm1[l]), sh1c, sc1c)
        if update_ctx:
            qc, kc, vc, bgc, cgc, uc = split_proj(hc @ w_in[l])
        else:
            kc, vc = jnp.split(hc @ w_in[l][:, D_ATTN:3 * D_ATTN], 2, axis=-1)
        kc = rms_norm(to_heads(kc), k_norm[l])
        vc = to_heads(vc)

        o_attn = neighbourhood_attention(q, k, v, kc, vc, rpb[l])
        o_conv = short_conv(u, bg, cg, w_conv[l])
        y = jnp.concatenate([group_rms(o_attn, g_attn_out[l]), group_rms(o_conv, g_conv_out[l])], axis=-1) @ w_out[l]

        if update_ctx:
            oc_attn = context_attention(rms_norm(to_heads(qc), q_norm[l]), kc, vc)
            oc_conv = short_conv(uc, bgc, cgc, w_conv[l])
            yc = jnp.concatenate([group_rms(oc_attn, g_attn_out[l]), group_rms(oc_conv, g_conv_out[l])], axis=-1) @ w_out[l]
            ctx = ctx + gt1c * yc
            h2c = modulate(rms_norm(ctx, g_norm2[l]), sh2c, sc2c)
            ctx = ctx + gt2c * peer(h2c, w_pq[l], sub_keys[l], u_experts[l], v_experts[l])

        x = x + gt1 * y
        h2 = modulate(rms_norm(x, g_norm2[l]), sh2, sc2)
        x = x + gt2 * peer(h2, w_pq[l], sub_keys[l], u_experts[l], v_experts[l])
    return x
```

```python
import os
from contextlib import ExitStack
import numpy as np
import concourse.bass as bass
import concourse.mybir as mybir
from concourse.bass_utils import run_bass_kernel_spmd

F32 = mybir.dt.float32
BF16 = mybir.dt.bfloat16
AF = mybir.ActivationFunctionType
ALU = mybir.AluOpType
AX = mybir.AxisListType

D = 2048
T = 2048
L = 256
TT = T + L
EPS = 1e-6
NTYPE = 21
NEG = -1e30


class Buf:
    __slots__ = ("name", "w", "r", "dsem", "dcnt", "ro")

    def __init__(self, name):
        self.name = name
        self.w = None
        self.r = []
        self.dsem = None
        self.dcnt = 0
        self.ro = False


class Sched:
    def __init__(self, nc, es):
        self.nc = nc
        self.es = es
        self.eng = {'pe': nc.tensor, 'dve': nc.vector, 'act': nc.scalar, 'pool': nc.gpsimd, 'sp': nc.sync}
        self.sem = {e: es.enter_context(nc.semaphore('s_' + e)) for e in ['pe', 'dve', 'act', 'pool']}
        self.cnt = {e: 0 for e in self.sem}
        self.seen = {e: {} for e in self.eng}
        self.dsems = []
        self.ninst = 0

    def buf(self, name):
        return Buf(name)

    def bufs(self, name, n):
        return [Buf(f"{name}{i}") for i in range(n)]

    def _wait(self, e, dep):
        kind, key, val = dep
        if kind == 'eng':
            if key == e and e == 'pe':
                return
            sem = self.sem[key]
            k = key
        else:
            sem = key[0]
            k = id(key[0])
        if self.seen[e].get(k, 0) >= val:
            return
        self.eng[e].wait_ge(sem, val)
        self.seen[e][k] = val

    def _deps(self, e, reads, writes, part=False):
        for b in reads:
            if b.w is not None:
                self._wait(e, b.w)
        for b in writes:
            if b.w is not None and not part:
                self._wait(e, b.w)
            for d in b.r:
                self._wait(e, d)

    def _mark(self, me, reads, writes):
        for b in reads:
            if not b.ro:
                b.r.append(me)
                if len(b.r) > 64:
                    last = {}
                    for d in b.r:
                        kk = d[1] if d[0] == 'eng' else d[1][1]
                        last[(d[0], kk)] = d
                    b.r = list(last.values())
        for b in writes:
            b.w = me
            b.r = []

    def op(self, e, fn, reads=(), writes=()):
        self._deps(e, reads, writes)
        ins = fn(self.eng[e])
        self.cnt[e] += 1
        self.ninst += 1
        ins.then_inc(self.sem[e], 1)
        self._mark(('eng', e, self.cnt[e]), reads, writes)
        return ins

    def dma(self, q, out, in_, reads=(), writes=(), owner=None, part=False, maxfly=None, **kw):
        self._deps(q, reads, writes, part)
        if owner is None:
            owner = writes[0] if writes else reads[0]
        if owner.dsem is None:
            owner.dsem = self.es.enter_context(self.nc.semaphore('d_' + owner.name))
            self.dsems.append(owner)
        if maxfly is not None and owner.dcnt >= 16 * maxfly:
            self._wait(q, ('dma', (owner.dsem, 'd_' + owner.name), owner.dcnt - 16 * (maxfly - 1)))
        ins = self.eng[q].dma_start(out=out, in_=in_, **kw)
        owner.dcnt += 16
        self.ninst += 1
        ins.then_inc(owner.dsem, 16)
        self._mark(('dma', (owner.dsem, 'd_' + owner.name), owner.dcnt), reads, writes)
        return ins

    def barrier(self, engines=('pe', 'dve', 'act', 'pool', 'sp')):
        for e in engines:
            for k in self.sem:
                if self.cnt[k] > 0:
                    self._wait(e, ('eng', k, self.cnt[k]))
            for o in self.dsems:
                if o.dcnt > 0:
                    self._wait(e, ('dma', (o.dsem, 'd_' + o.name), o.dcnt))


def _attn_types():
    types = [(8, 8 + d) for d in (-2, -1, 0, 1, 2)]
    for qb in (0, 1):
        types += [(qb, kt) for kt in range(4)]
    for qb in (14, 15):
        types += [(qb, kt) for kt in range(12, 16)]
    return types


def _qb_plan(qb):
    if 2 <= qb <= 13:
        return 0, [qb + d for d in (-2, -1, 0, 1, 2)]
    ei = {0: 0, 1: 1, 14: 2, 15: 3}[qb]
    kts = list(range(4)) if qb < 2 else list(range(12, 16))
    return 5 + 4 * ei, kts


def _bias_tables(rpb):
    types = _attn_types()
    key = np.arange(128)
    q = np.arange(128)
    rkl, kc = key // 64, key % 64
    rl, qc = q // 64, q % 64
    biasT = np.zeros((8, 128, NTYPE, 128), np.float32)
    maskT = np.zeros((128, NTYPE, 128), np.float32)
    for ti, (qb, kt) in enumerate(types):
        r = 2 * qb + rl
        rk = 2 * kt + rkl
        rs = np.clip(r - 4, 0, 24)
        rowv = (rk[:, None] >= rs[None, :]) & (rk[:, None] < rs[None, :] + 8)
        cs = np.clip(qc - 8, 0, 48)
        colv = (kc[:, None] >= cs[None, :]) & (kc[:, None] < cs[None, :] + 16)
        dr = np.clip(rk[:, None] - r[None, :] + 7, 0, 14)
        dc = np.clip(kc[:, None] - qc[None, :], -15, 15) + 15
        biasT[:, :, ti, :] = rpb[:, dr, dc]
        maskT[:, ti, :] = (rowv & colv).astype(np.float32)
    return biasT, maskT


G_G1, G_G2, G_QN, G_KN, G_GATT, G_GCONV, G_WC, G_N = 0, 16, 32, 33, 34, 42, 50, 74


def _host_inputs(inp, b):
    f = np.float32
    m = {}
    m["x"] = np.ascontiguousarray(inp["x"][b], f)
    m["ctx"] = np.ascontiguousarray(inp["ctx"][b], f)
    cc = np.stack([inp["c"][b].reshape(16, 128).T, inp["c_ctx"].reshape(16, 128).T], axis=-1)
    m["cc"] = np.ascontiguousarray(cc, f)
    m["w_ada"] = np.ascontiguousarray(inp["w_ada"][0], f)
    m["b_adaT"] = np.ascontiguousarray(inp["b_ada"][0].reshape(96, 128).T, f)
    gv = np.zeros((128, G_N), f)
    gv[:, G_G1:G_G1 + 16] = inp["g_norm1"][0].reshape(16, 128).T
    gv[:, G_G2:G_G2 + 16] = inp["g_norm2"][0].reshape(16, 128).T
    gv[:, G_QN] = inp["q_norm"][0]
    gv[:, G_KN] = inp["k_norm"][0]
    gv[:, G_GATT:G_GATT + 8] = inp["g_attn_out"][0].T
    gv[:, G_GCONV:G_GCONV + 8] = inp["g_conv_out"][0].T
    gv[:, G_WC:G_WC + 24] = inp["w_conv"][0].reshape(3, 8, 128).transpose(2, 1, 0).reshape(128, 24)
    m["gvec"] = gv
    m["w_in"] = np.ascontiguousarray(inp["w_in"][0], f)
    m["w_out"] = np.ascontiguousarray(inp["w_out"][0], f)
    m["w_pq"] = np.ascontiguousarray(inp["w_pq"][0], f)
    m["skT"] = np.ascontiguousarray(inp["sub_keys"][0].reshape(16, 128, 128).transpose(2, 0, 1), f)
    m["u_tab"] = np.ascontiguousarray(inp["u_experts"][0], f)
    m["v_tab"] = np.ascontiguousarray(inp["v_experts"][0], f)
    biasT, maskT = _bias_tables(np.asarray(inp["rpb"][0], f))
    m["biasT"] = biasT
    m["maskT"] = maskT
    m["ident"] = np.eye(128, dtype=f)
    return m


def build_program(stop_after=None, dbg=False):
    nc = bass.Bass("TRN2", target_bir_lowering=False)

    def din(name, shape, dt=F32):
        return nc.dram_tensor(name, list(shape), dt, kind="ExternalInput").ap()

    def dscr(name, shape, dt):
        return nc.dram_tensor(name, list(shape), dt, kind="Internal").ap()

    x = din("x", [T, D]); ctx = din("ctx", [L, D]); cc = din("cc", [128, 16, 2])
    w_ada = din("w_ada", [D, 6 * D]); b_adaT = din("b_adaT", [128, 96]); gvec = din("gvec", [128, G_N])
    w_in = din("w_in", [D, 6144]); w_out = din("w_out", [D, D]); w_pq = din("w_pq", [D, D])
    skT = din("skT", [128, 16, 128]); u_tab = din("u_tab", [16384, D]); v_tab = din("v_tab", [16384, D])
    biasT = din("biasT", [8, 128, NTYPE, 128]); maskT = din("maskT", [128, NTYPE, 128]); ident = din("ident", [128, 128])
    out = nc.dram_tensor("out", [T, D], F32, kind="ExternalOutput").ap()

    uT_d = dscr("uT_d", [128, 128, D], BF16)
    cat_d = dscr("cat_d", [16, 128, T], BF16)
    x1_d = dscr("x1_d", [T, D], F32)
    h2_d = dscr("h2_d", [128, 16, T], BF16)
    s_d = dscr("s_d", [16, T, 128], F32)
    GT_d = dscr("GT_d", [128, 128, T], BF16)
    gt_d = dscr("gt_d", [32, 128], F32)
    dbg_out = None
    if dbg:
        dbg_out = {
            "d_mod": nc.dram_tensor("d_mod", [128, 96, 2], F32, kind="ExternalOutput").ap(),
            "d_hT": nc.dram_tensor("d_hT", [128, 16, TT], BF16, kind="ExternalOutput").ap(),
            "d_cat": nc.dram_tensor("d_cat", [16, 128, T], BF16, kind="ExternalOutput").ap(),
            "d_x1": nc.dram_tensor("d_x1", [T, D], F32, kind="ExternalOutput").ap(),
            "d_h2": nc.dram_tensor("d_h2", [128, 16, T], BF16, kind="ExternalOutput").ap(),
            "d_s": nc.dram_tensor("d_s", [T, 2048], F32, kind="ExternalOutput").ap(),
            "d_GT": nc.dram_tensor("d_GT", [128, 128, T], BF16, kind="ExternalOutput").ap(),
        }

    with ExitStack() as es:
        S = Sched(nc, es)
        PS = es.enter_context(nc.psum_tensor("PS", [128, 4096], F32))
        PB = S.bufs("psb", 8)

        def bank(i, n=512):
            return PS[:, i * 512:i * 512 + n]

        def sbt(stack, name, shape, dt):
            return stack.enter_context(nc.sbuf_tensor(name, list(shape), dt))

        idf = sbt(es, "idf", [128, 128], F32); idb = sbt(es, "idb", [128, 128], BF16)
        onesb = sbt(es, "onesb", [128, 128], BF16)
        gv = sbt(es, "gv", [128, G_N], F32)
        modT = sbt(es, "modT", [128, 96, 2], F32)
        der = sbt(es, "der", [128, 6, 16], F32)
        gt12 = sbt(es, "gt12", [128, 32], F32)
        epsb = sbt(es, "epsb", [128, 1], F32)
        Bc = S.buf("consts"); Bmod = S.buf("modT"); Bder = S.buf("der"); Bgtbc = S.buf("gtbc")

        S.dma('sp', idf[:], ident, writes=[Bc])
        S.dma('sp', gv[:], gvec, writes=[Bc])
        S.op('dve', lambda e: e.tensor_copy(out=idb[:], in_=idf[:]), reads=[Bc], writes=[Bc])
        S.op('dve', lambda e: e.memset(onesb[:], 1.0), writes=[Bc])
        S.op('dve', lambda e: e.memset(epsb[:], EPS), writes=[Bc])

        def rstd_from_ss(ss_ap, out_ap, tmp_ap, n_inv, reads, writes, tmpbuf):
            S.op('act', lambda e: e.activation(out=tmp_ap, in_=ss_ap, func=AF.Sqrt, scale=n_inv, bias=epsb[:ss_ap.shape[0], 0:1]),
                 reads=list(reads) + [Bc], writes=[tmpbuf])
            S.op('dve', lambda e: e.reciprocal(out=out_ap, in_=tmp_ap), reads=[tmpbuf], writes=writes)

        BuT = S.buf("uT_d")
        if stop_after == "T":
            return nc, S

        with ExitStack() as ph:
            ccs = sbt(ph, "ccs", [128, 16, 2], F32)
            ccr = sbt(ph, "ccr", [128, 16, 2], F32)
            bad = sbt(ph, "bad", [128, 96], F32)
            wa = [sbt(ph, f"wa{i}", [128, 16, 512], F32) for i in range(2)]
            Bwa = S.bufs("wa", 2); Bcc = S.buf("cc")
            S.dma('sp', ccr[:], cc, writes=[Bcc])
            S.dma('sp', bad[:], b_adaT, writes=[Bcc])
            S.op('act', lambda e: e.activation(out=ccs[:], in_=ccr[:], func=AF.Silu), reads=[Bcc], writes=[Bcc])
            w_ada_v = w_ada.rearrange("(k p) f -> p k f", p=128)

            def load_wa(fb):
                S.dma('sp', wa[fb % 2][:], w_ada_v[:, :, fb * 512:(fb + 1) * 512], writes=[Bwa[fb % 2]])
            load_wa(0)
            mps = bank(0, 192).rearrange("p (j two) -> p j two", two=2)
            for fb in range(24):
                if fb + 1 < 24:
                    load_wa(fb + 1)
                for m in range(4):
                    j = fb * 4 + m
                    for k in range(16):
                        S.op('pe', lambda e: e.matmul(mps[:, j, :], lhsT=wa[fb % 2][:, k, m * 128:(m + 1) * 128], rhs=ccs[:, k, :],
                                                      start=(k == 0), stop=(k == 15)),
                             reads=[Bwa[fb % 2], Bcc], writes=[PB[0]])
            S.op('dve', lambda e: e.tensor_tensor(out=modT[:], in0=mps, in1=bad[:].unsqueeze(2).to_broadcast([128, 96, 2]), op=ALU.add),
                 reads=[PB[0], Bcc], writes=[Bmod])
            g1 = gv[:, G_G1:G_G1 + 16]; g2 = gv[:, G_G2:G_G2 + 16]
            for (dst, sc_m, col, g) in ((0, 1, 0, g1), (2, 1, 1, g1), (4, 4, 0, g2)):
                S.op('dve', lambda e: e.scalar_tensor_tensor(out=der[:, dst, :], in0=modT[:, sc_m * 16:(sc_m + 1) * 16, col], scalar=1.0, in1=g,
                                                             op0=ALU.add, op1=ALU.mult), reads=[Bmod, Bc], writes=[Bder])
            for (dst, sh_m, col) in ((1, 0, 0), (3, 0, 1), (5, 3, 0)):
                S.op('dve', lambda e: e.tensor_copy(out=der[:, dst, :], in_=modT[:, sh_m * 16:(sh_m + 1) * 16, col]), reads=[Bmod], writes=[Bder])
            S.op('dve', lambda e: e.tensor_copy(out=gt12[:, 0:16], in_=modT[:, 32:48, 0]), reads=[Bmod], writes=[Bder])
            S.op('dve', lambda e: e.tensor_copy(out=gt12[:, 16:32], in_=modT[:, 80:96, 0]), reads=[Bmod], writes=[Bder])
            if dbg:
                S.dma('sp', dbg_out["d_mod"], modT[:], reads=[Bmod], writes=[S.buf("dbgm")])
            S.barrier()
        if stop_after == "0":
            return nc, S

        A1x, sh1x, A1c, sh1c, A2, sh2 = [der[:, i, :] for i in range(6)]

        def norm_transpose(ph_bufs, xt_ap, Bxt, A_ap, sh_ap, dst_fn, Bdst, pbanks, extra_reads=()):
            junk, Bjunk, ssq, rt, rstd, Bst, xs, Bxs = ph_bufs
            S.op('act', lambda e: e.activation(out=junk[:], in_=xt_ap, func=AF.Square, accum_out=ssq[:]), reads=[Bxt], writes=[Bjunk, Bst])
            rstd_from_ss(ssq[:], rstd[:], rt[:], 1.0 / D, [Bst], [Bst], Bst)
            S.op('dve', lambda e: e.tensor_scalar(out=xs[:], in0=xt_ap, scalar1=rstd[:, 0:1], scalar2=None, op0=ALU.mult), reads=[Bxt, Bst], writes=[Bxs])
            for half in range(2):
                pb = pbanks[half]
                psb = bank(pb).bitcast(BF16)
                for j in range(8):
                    k = half * 8 + j
                    S.op('pe', lambda e: e.transpose(out=psb[:, j * 128:(j + 1) * 128], in_=xs[:, k * 128:(k + 1) * 128], identity=idb[:]),
                         reads=[Bxs, Bc], writes=[PB[pb]])
                for j in range(8):
                    k = half * 8 + j
                    if j % 2 == 0:
                        S.op('act', lambda e: e.activation(out=dst_fn(k), in_=psb[:, j * 128:(j + 1) * 128], func=AF.Identity,
                                                           scale=A_ap[:, k:k + 1], bias=sh_ap[:, k:k + 1]),
                             reads=[PB[pb], Bder] + list(extra_reads), writes=[Bdst])
                    else:
                        S.op('dve', lambda e: e.tensor_scalar(out=dst_fn(k), in0=psb[:, j * 128:(j + 1) * 128], scalar1=A_ap[:, k:k + 1],
                                                              scalar2=sh_ap[:, k:k + 1], op0=ALU.mult, op1=ALU.add),
                             reads=[PB[pb], Bder] + list(extra_reads), writes=[Bdst])

        def make_nt_bufs(ph, tag):
            junk = sbt(ph, "junk" + tag, [128, D], BF16); ssq = sbt(ph, "ssq" + tag, [128, 1], F32)
            rt = sbt(ph, "rt" + tag, [128, 1], F32); rstd = sbt(ph, "rstd" + tag, [128, 1], F32)
            xs = sbt(ph, "xs" + tag, [128, D], BF16)
            return (junk, S.buf("junk" + tag), ssq, rt, rstd, S.buf("st" + tag), xs, S.buf("xs" + tag))

        Bcat = S.buf("cat_d")
        with ExitStack() as ph:
            hT = sbt(ph, "hT", [128, 16, TT], BF16)
            BhT = S.buf("hT")
            with ExitStack() as p1a:
                xt = [sbt(p1a, f"xt{i}", [128, D], F32) for i in range(2)]
                Bxt = S.bufs("xt", 2)
                ntb = make_nt_bufs(p1a, "a")

                def load_x(i):
                    src = x[i * 128:(i + 1) * 128, :] if i < 16 else ctx[(i - 16) * 128:(i - 15) * 128, :]
                    S.dma('sp', xt[i % 2][:], src, writes=[Bxt[i % 2]])
                load_x(0)
                for i in range(18):
                    if i + 1 < 18:
                        load_x(i + 1)
                    A_ap, sh_ap = (A1x, sh1x) if i < 16 else (A1c, sh1c)
                    norm_transpose(ntb, xt[i % 2][:], Bxt[i % 2], A_ap, sh_ap,
                                   lambda k: hT[:, k, i * 128:(i + 1) * 128], BhT, (0, 1))
                S.barrier()
            if dbg:
                S.dma('sp', dbg_out["d_hT"], hT[:], reads=[BhT], writes=[S.buf("dbgh")])
            BhT.ro = True

            w_in_v = w_in.rearrange("(k p) f -> p k f", p=128)
            wf = [sbt(ph, f"wf{i}", [128, 16, 128], F32) for i in range(3)]
            wb = [sbt(ph, f"wb{i}", [128, 16, 128], BF16) for i in range(3)]
            Bwf = S.bufs("wf", 3); Bwb = S.bufs("wb", 3)
            sqb = sbt(ph, "sqb", [128, 512], BF16); Bsq = S.buf("sqb")
            rtt = sbt(ph, "rtt", [128, 512], F32); Brtt = S.buf("rtt")
            rsd = sbt(ph, "rsd", [128, 512], F32); Brsd = S.buf("rsd")
            catb = [sbt(ph, f"catb{i}", [128, T], BF16) for i in range(2)]
            Bcatb = S.bufs("catb", 2)

            def load_w(slot, col0):
                S.dma('sp', wf[slot][:], w_in_v[:, :, col0:col0 + 128], writes=[Bwf[slot]])

            def cast_w(slot, eng):
                if eng == 'act':
                    S.op('act', lambda e: e.activation(out=wb[slot][:], in_=wf[slot][:], func=AF.Copy), reads=[Bwf[slot]], writes=[Bwb[slot]])
                else:
                    S.op(eng, lambda e: e.tensor_copy(out=wb[slot][:], in_=wf[slot][:]), reads=[Bwf[slot]], writes=[Bwb[slot]])

            def group_norm_T(ps_ap, n, gcol_ap, dst_ap, Bdst, src_reads, pb_ss):
                S.op('act', lambda e: e.activation(out=sqb[:, :n], in_=ps_ap, func=AF.Square), reads=src_reads, writes=[Bsq])
                S.op('pe', lambda e: e.matmul(bank(pb_ss, n), lhsT=onesb[:], rhs=sqb[:, :n], start=True, stop=True), reads=[Bsq, Bc], writes=[PB[pb_ss]])
                rstd_from_ss(bank(pb_ss, n), rsd[:, :n], rtt[:, :n], 1.0 / 128, [PB[pb_ss]], [Brsd], Brtt)
                S.op('dve', lambda e: e.scalar_tensor_tensor(out=dst_ap, in0=ps_ap, scalar=gcol_ap, in1=rsd[:, :n], op0=ALU.mult, op1=ALU.mult),
                     reads=list(src_reads) + [Brsd, Bc], writes=[Bdst])

            with ExitStack() as p1b:
                QT = sbt(p1b, "QT", [128, T], BF16); KT = sbt(p1b, "KT", [128, TT], BF16)
                Vh = sbt(p1b, "Vh", [128, 18, 128], BF16)
                BQT = S.buf("QT"); BKT = S.buf("KT"); BVh = S.buf("Vh")
                bia = sbt(p1b, "bia", [128, NTYPE, 128], F32); msk = sbt(p1b, "msk", [128, NTYPE, 128], F32)
                Eh = sbt(p1b, "Eh", [128, NTYPE, 128], BF16)
                Bbia = S.buf("bia"); Bmsk = S.buf("msk"); BEh = S.buf("Eh")
                pa_es = [sbt(p1b, f"pa_e{i}", [128, 512], F32) for i in range(2)]; Bpaes = S.bufs("pae", 2)
                pb_es = [sbt(p1b, f"pb_e{i}", [128, 128], F32) for i in range(2)]; Bpbes = S.bufs("pbe", 2)
                PA = [sbt(p1b, f"PA{i}", [128, 512], BF16) for i in range(2)]
                PBt = [sbt(p1b, f"PBt{i}", [128, 384], BF16) for i in range(2)]
                BPA = S.bufs("PA", 2); BPBt = S.bufs("PBt", 2)
                rden = sbt(p1b, "rden", [128, 512], F32); Brden = S.buf("rden")
                osb = sbt(p1b, "osb", [128, 512], F32); Bosb = S.buf("osb")
                oraw = sbt(p1b, "oraw", [128, 512], F32); Boraw = S.buf("oraw")
                S.dma('sp', msk[:], maskT, writes=[Bmsk])
                scale = 128.0 ** -0.5
                def head_prologue(h):
                    load_w(0, 128 * h); load_w(1, 1024 + 128 * h); load_w(2, 2048 + 128 * h)
                    S.dma('sp', bia[:], biasT[h], writes=[Bbia])
                    cast_w(0, 'pool'); cast_w(1, 'pool'); cast_w(2, 'pool')
                    S.op('act', lambda e: e.activation(out=bia[:], in_=bia[:], func=AF.Exp), reads=[Bbia], writes=[Bbia])
                head_prologue(0)
                for h in range(8):
                    S.op('dve', lambda e: e.tensor_tensor(out=Eh[:], in0=bia[:], in1=msk[:], op=ALU.mult), reads=[Bbia, Bmsk], writes=[BEh])
                    for (slot, dst, Bd, ngrp, gcol) in ((0, QT, BQT, 4, G_QN), (1, KT, BKT, 5, G_KN)):
                        for g in range(ngrp):
                            n = 512 if g < 4 else 256
                            pb = g % 2
                            for k in range(16):
                                S.op('pe', lambda e: e.matmul(bank(pb, n), lhsT=wb[slot][:, k, :], rhs=hT[:, k, g * 512:g * 512 + n],
                                                              start=(k == 0), stop=(k == 15)), reads=[Bwb[slot], BhT], writes=[PB[pb]])
                            group_norm_T(bank(pb, n), n, gv[:, gcol:gcol + 1], dst[:, g * 512:g * 512 + n], Bd, [PB[pb]], 2)
                    for g in range(5):
                        pb = g % 2
                        nt = 4 if g < 4 else 2
                        for j in range(nt):
                            ti = g * 4 + j
                            for k in range(16):
                                S.op('pe', lambda e: e.matmul(bank(pb)[:, j * 128:(j + 1) * 128], lhsT=hT[:, k, ti * 128:(ti + 1) * 128], rhs=wb[2][:, k, :],
                                                              start=(k == 0), stop=(k == 15)), reads=[Bwb[2], BhT], writes=[PB[pb]])
                        S.op('act', lambda e: e.activation(out=Vh[:, g * 4:g * 4 + nt, :].rearrange("p a b -> p (a b)"), in_=bank(pb, nt * 128), func=AF.Copy),
                             reads=[PB[pb]], writes=[BVh])
                    if h + 1 < 8:
                        head_prologue(h + 1)
                    cb = catb[h % 2]; Bcb = Bcatb[h % 2]
                    def emit_S(qb):
                        t0, kts = _qb_plan(qb)
                        nw = len(kts)
                        i2 = qb % 2
                        bA, bB = (3, 4) if i2 == 0 else (0, 1)
                        pa_e = pa_es[i2]; Bpae = Bpaes[i2]; pb_e = pb_es[i2]; Bpbe = Bpbes[i2]
                        qs = QT[:, qb * 128:(qb + 1) * 128]
                        for j in range(4):
                            S.op('pe', lambda e: e.matmul(bank(bA)[:, j * 128:(j + 1) * 128], lhsT=KT[:, kts[j] * 128:(kts[j] + 1) * 128], rhs=qs, start=True, stop=True),
                                 reads=[BKT, BQT], writes=[PB[bA]])
                        ktb = ([kts[4]] if nw == 5 else []) + [16, 17]
                        for j, kt in enumerate(ktb):
                            S.op('pe', lambda e: e.matmul(bank(bB)[:, j * 128:(j + 1) * 128], lhsT=KT[:, kt * 128:(kt + 1) * 128], rhs=qs, start=True, stop=True),
                                 reads=[BKT, BQT], writes=[PB[bB]])
                        S.op('act', lambda e: e.activation(out=pa_e[:], in_=bank(bA), func=AF.Exp, scale=scale), reads=[PB[bA]], writes=[Bpae])
                        S.op('dve', lambda e: e.tensor_tensor(out=PA[i2][:], in0=pa_e[:], in1=Eh[:, t0:t0 + 4, :].rearrange("p a b -> p (a b)"), op=ALU.mult),
                             reads=[Bpae, BEh], writes=[BPA[i2]])
                        if nw == 5:
                            S.op('act', lambda e: e.activation(out=pb_e[:], in_=bank(bB, 128), func=AF.Exp, scale=scale), reads=[PB[bB]], writes=[Bpbe])
                            S.op('dve', lambda e: e.tensor_tensor(out=PBt[i2][:, 0:128], in0=pb_e[:], in1=Eh[:, t0 + 4, :], op=ALU.mult),
                                 reads=[Bpbe, BEh], writes=[BPBt[i2]])
                            S.op('act', lambda e: e.activation(out=PBt[i2][:, 128:384], in_=bank(bB)[:, 128:384], func=AF.Exp, scale=scale), reads=[PB[bB]], writes=[BPBt[i2]])
                        else:
                            S.op('act', lambda e: e.activation(out=PBt[i2][:, 0:256], in_=bank(bB)[:, 0:256], func=AF.Exp, scale=scale), reads=[PB[bB]], writes=[BPBt[i2]])
                        return [(PA[i2][:, j * 128:(j + 1) * 128], kts[j], BPA[i2]) for j in range(4)] + \
                               [(PBt[i2][:, j * 128:(j + 1) * 128], kt, BPBt[i2]) for j, kt in enumerate(ktb)]

                    plist_next = emit_S(0)
                    for qb in range(16):
                        qg, ql = qb // 4, qb % 4
                        plist = plist_next
                        if qb + 1 < 16:
                            plist_next = emit_S(qb + 1)
                        bD = 6 + (qg % 2)
                        np_ = len(plist)
                        for j, (pap, kt, Bp) in enumerate(plist):
                            S.op('pe', lambda e: e.matmul(bank(5)[:, ql * 128:(ql + 1) * 128], lhsT=Vh[:, kt, :], rhs=pap, start=(j == 0), stop=(j == np_ - 1)),
                                 reads=[BVh, Bp], writes=[PB[5]])
                        for j, (pap, kt, Bp) in enumerate(plist):
                            S.op('pe', lambda e: e.matmul(bank(bD)[:, ql * 128:(ql + 1) * 128], lhsT=onesb[:], rhs=pap, start=(j == 0), stop=(j == np_ - 1)),
                                 reads=[Bc, Bp], writes=[PB[bD]])
                        if ql == 3:
                            S.op('act', lambda e: e.activation(out=oraw[:], in_=bank(5), func=AF.Copy), reads=[PB[5]], writes=[Boraw])
                            S.op('dve', lambda e: e.reciprocal(out=rden[:], in_=bank(bD)), reads=[PB[bD]], writes=[Brden])
                            S.op('dve', lambda e: e.tensor_tensor(out=osb[:], in0=oraw[:], in1=rden[:], op=ALU.mult), reads=[Boraw, Brden], writes=[Bosb])
                            group_norm_T(osb[:], 512, gv[:, G_GATT + h:G_GATT + h + 1], cb[:, qg * 512:(qg + 1) * 512], Bcb, [Bosb], 2)
                    S.dma('act', cat_d[h], cb[:], reads=[Bcb], writes=[Bcat], owner=Bcb, part=True)
                S.barrier()
            if stop_after == "1b":
                return nc, S

            with ExitStack() as p1c:
                zb = sbt(p1c, "zb", [128, T + 2], F32); bgs = sbt(p1c, "bgs", [128, T], F32)
                cgs = sbt(p1c, "cgs", [128, 512], F32); yb = sbt(p1c, "yb", [128, T], F32)
                Bzb = S.buf("zb"); Bbgs = S.buf("bgs"); Bcgs = S.buf("cgs"); Byb = S.buf("yb")
                S.op('dve', lambda e: e.memset(zb[:, 0:1], 0.0), writes=[Bzb])
                S.op('dve', lambda e: e.memset(zb[:, T + 1:T + 2], 0.0), writes=[Bzb])
                for j in range(8):
                    load_w(0, 3072 + 128 * j); load_w(1, 4096 + 128 * j); load_w(2, 5120 + 128 * j)
                    cast_w(0, 'pool'); cast_w(1, 'pool'); cast_w(2, 'pool')
                    for g in range(4):
                        for slot, pb in ((0, 0), (1, 1), (2, 3)):
                            for k in range(16):
                                S.op('pe', lambda e: e.matmul(bank(pb), lhsT=wb[slot][:, k, :], rhs=hT[:, k, g * 512:(g + 1) * 512],
                                                              start=(k == 0), stop=(k == 15)), reads=[Bwb[slot], BhT], writes=[PB[pb]])
                        S.op('act', lambda e: e.activation(out=bgs[:, g * 512:(g + 1) * 512], in_=bank(0), func=AF.Copy), reads=[PB[0]], writes=[Bbgs])
                        S.op('act', lambda e: e.activation(out=cgs[:], in_=bank(1), func=AF.Copy), reads=[PB[1]], writes=[Bcgs])
                        S.op('dve', lambda e: e.tensor_tensor(out=zb[:, 1 + g * 512:1 + (g + 1) * 512], in0=bank(3), in1=cgs[:], op=ALU.mult),
                             reads=[PB[3], Bcgs], writes=[Bzb])
                    wc = lambda i: gv[:, G_WC + 3 * j + i:G_WC + 3 * j + i + 1]
                    S.op('dve', lambda e: e.tensor_scalar(out=yb[:], in0=zb[:, 1:T + 1], scalar1=wc(1), scalar2=None, op0=ALU.mult), reads=[Bzb, Bc], writes=[Byb])
                    S.op('dve', lambda e: e.scalar_tensor_tensor(out=yb[:], in0=zb[:, 0:T], scalar=wc(0), in1=yb[:], op0=ALU.mult, op1=ALU.add), reads=[Bzb, Bc, Byb], writes=[Byb])
                    S.op('dve', lambda e: e.scalar_tensor_tensor(out=yb[:], in0=zb[:, 2:T + 2], scalar=wc(2), in1=yb[:], op0=ALU.mult, op1=ALU.add), reads=[Bzb, Bc, Byb], writes=[Byb])
                    S.op('pool', lambda e: e.tensor_tensor(out=yb[:], in0=yb[:], in1=bgs[:], op=ALU.mult), reads=[Byb, Bbgs], writes=[Byb])
                    cb = catb[j % 2]; Bcb = Bcatb[j % 2]
                    for g in range(4):
                        group_norm_T(yb[:, g * 512:(g + 1) * 512], 512, gv[:, G_GCONV + j:G_GCONV + j + 1], cb[:, g * 512:(g + 1) * 512], Bcb, [Byb], 2)
                    S.dma('act', cat_d[8 + j], cb[:], reads=[Bcb], writes=[Bcat], owner=Bcb, part=True)
                S.barrier()
        if dbg:
            with ExitStack() as phd:
                tmpc = sbt(phd, "tmpc", [128, T], BF16); Btc = S.buf("tmpc")
                for i in range(16):
                    S.dma('sp', tmpc[:], cat_d[i], reads=[Bcat], writes=[Btc])
                    S.dma('sp', dbg_out["d_cat"][i], tmpc[:], reads=[Btc], writes=[S.buf("dbgc")], owner=Btc)
                S.barrier()
        if stop_after == "1":
            return nc, S

        gt1bc = None
        gt2bc = sbt(es, "gt2bc", [128, D], F32)
        def make_gtbc(ph):
            g1 = sbt(ph, "gt1bc", [128, D], F32)
            gtr = sbt(ph, "gtr", [32, 128], F32)
            Bgtr = S.buf("gtr"); Bgtd = S.buf("gt_d")
            S.op('pe', lambda e: e.transpose(out=bank(1)[:32, 0:128], in_=gt12[:, :], identity=idf[:]), reads=[Bder, Bc], writes=[PB[1]])
            S.op('dve', lambda e: e.tensor_copy(out=gtr[:], in_=bank(1)[:32, 0:128]), reads=[PB[1]], writes=[Bgtr])
            S.dma('sp', gt_d, gtr[:], reads=[Bgtr], writes=[Bgtd])
            for m, dst in ((0, g1), (1, gt2bc)):
                src = gt_d[m * 16:(m + 1) * 16, :].rearrange("k p -> (k p)").partition_broadcast(128)
                S.dma('sp', dst[:], src, reads=[Bgtd], writes=[Bgtbc], part=(m == 1))
            return g1

        Bx1 = S.buf("x1_d"); Bh2 = S.buf("h2_d")
        with ExitStack() as ph:
            gt1bc = make_gtbc(ph)
            wo = sbt(ph, "wo", [128, 16, D], BF16); Bwo = S.buf("wo")
            w_out_v = w_out.rearrange("(k p) f -> p k f", p=128)
            with ExitStack() as p3a:
                wof = [sbt(p3a, f"wof{i}", [128, 16, 128], F32) for i in range(2)]; Bwof = S.bufs("wof", 2)
                for i in range(16):
                    S.dma('sp', wof[i % 2][:], w_out_v[:, :, i * 128:(i + 1) * 128], writes=[Bwof[i % 2]])
                    eng = 'dve' if i % 2 == 0 else 'pool'
                    S.op(eng, lambda e: e.tensor_copy(out=wo[:, :, i * 128:(i + 1) * 128], in_=wof[i % 2][:]), reads=[Bwof[i % 2]], writes=[Bwo])
                S.barrier()
            Bwo.ro = True
            catg = [sbt(ph, f"catg{i}", [128, 16, 512], BF16) for i in range(2)]; Bcatg = S.bufs("catg", 2)
            xt = [sbt(ph, f"xt3{i}", [128, D], F32) for i in range(2)]; Bxt = S.bufs("xt3", 2)
            ytmp = sbt(ph, "ytmp", [128, D], F32); Bytmp = S.buf("ytmp")
            h2g = sbt(ph, "h2g", [128, 16, 512], BF16); Bh2g = S.buf("h2g")
            ntb = make_nt_bufs(ph, "c")

            def load_catg(g):
                S.dma('sp', catg[g % 2][:], cat_d[:, :, g * 512:(g + 1) * 512].rearrange("k p t -> p k t"), reads=[Bcat], writes=[Bcatg[g % 2]])
            load_catg(0)
            S.dma('sp', xt[0][:], x[0:128, :], writes=[Bxt[0]])
            ypsum = PS[:, 2048:4096]
            for ti in range(16):
                g = ti // 4
                if ti % 4 == 0 and g + 1 < 4:
                    load_catg(g + 1)
                if ti + 1 < 16:
                    S.dma('sp', xt[(ti + 1) % 2][:], x[(ti + 1) * 128:(ti + 2) * 128, :], writes=[Bxt[(ti + 1) % 2]])
                i2 = ti % 2
                cg_ = catg[g % 2]; Bcg_ = Bcatg[g % 2]
                tl = (ti % 4) * 128
                for k in range(16):
                    for db in range(4):
                        S.op('pe', lambda e: e.matmul(bank(4 + db), lhsT=cg_[:, k, tl:tl + 128], rhs=wo[:, k, db * 512:(db + 1) * 512],
                                                      start=(k == 0), stop=(k == 15)), reads=[Bcg_, Bwo], writes=[PB[4 + db]])
                S.op('dve', lambda e: e.tensor_tensor(out=ytmp[:], in0=ypsum, in1=gt1bc[:], op=ALU.mult),
                     reads=[PB[4], PB[5], PB[6], PB[7], Bgtbc], writes=[Bytmp])
                S.op('pool', lambda e: e.tensor_tensor(out=xt[i2][:], in0=ytmp[:], in1=xt[i2][:], op=ALU.add), reads=[Bytmp, Bxt[i2]], writes=[Bxt[i2]])
                S.dma('act', x1_d[ti * 128:(ti + 1) * 128, :], xt[i2][:], reads=[Bxt[i2]], writes=[Bx1], owner=Bxt[i2], part=True)
                norm_transpose(ntb, xt[i2][:], Bxt[i2], A2, sh2,
                               lambda k: h2g[:, k, tl:tl + 128], Bh2g, (0, 1))
                if ti % 4 == 3:
                    S.dma('act', h2_d[:, :, g * 512:(g + 1) * 512], h2g[:], reads=[Bh2g], writes=[Bh2], owner=Bh2g, part=True)
            S.barrier()
        if dbg:
            with ExitStack() as phd:
                tmpx = sbt(phd, "tmpx", [128, D], F32); Btx = S.buf("tmpx")
                for i in range(16):
                    S.dma('sp', tmpx[:], x1_d[i * 128:(i + 1) * 128, :], reads=[Bx1], writes=[Btx])
                    S.dma('sp', dbg_out["d_x1"][i * 128:(i + 1) * 128, :], tmpx[:], reads=[Btx], writes=[S.buf("dbgx")], owner=Btx)
                tmph = sbt(phd, "tmph", [128, 16, 512], BF16); Bth = S.buf("tmph")
                for g in range(4):
                    S.dma('sp', tmph[:], h2_d[:, :, g * 512:(g + 1) * 512], reads=[Bh2], writes=[Bth])
                    S.dma('sp', dbg_out["d_h2"][:, :, g * 512:(g + 1) * 512], tmph[:], reads=[Bth], writes=[S.buf("dbgh2")], owner=Bth)
                S.barrier()
        if stop_after == "3":
            return nc, S

        Bsd = S.buf("s_d")
        with ExitStack() as ph:
            h2T = sbt(ph, "h2T", [128, 16, T], BF16); Bh2T = S.buf("h2T")
            for g in range(4):
                S.dma('sp', h2T[:, :, g * 512:(g + 1) * 512], h2_d[:, :, g * 512:(g + 1) * 512], reads=[Bh2], writes=[Bh2T], part=(g > 0))
            Bh2T.ro = True
            skt = sbt(ph, "skt", [128, 16, 128], F32); Bsk = S.buf("skt")
            S.dma('sp', skt[:], skT, writes=[Bsk])
            Bsk.ro = True
            w_pq_v = w_pq.rearrange("(k p) f -> p k f", p=128)
            wf = [sbt(ph, f"wpf{i}", [128, 16, 128], F32) for i in range(2)]; Bwf = S.bufs("wpf", 2)
            wb = [sbt(ph, f"wpb{i}", [128, 16, 128], BF16) for i in range(2)]; Bwb = S.bufs("wpb", 2)
            qTs = [sbt(ph, f"qTs{i}", [128, 512], F32) for i in range(2)]; BqTs = S.bufs("qTs", 2)
            ssb = [sbt(ph, f"ssb{i}", [128, 512], F32) for i in range(2)]; Bssb = S.bufs("ssb", 2)
            S.dma('sp', wf[0][:], w_pq_v[:, :, 0:128], writes=[Bwf[0]])
            cnt = 0
            for j in range(16):
                if j + 1 < 16:
                    S.dma('sp', wf[(j + 1) % 2][:], w_pq_v[:, :, (j + 1) * 128:(j + 2) * 128], writes=[Bwf[(j + 1) % 2]])
                S.op('pool', lambda e: e.tensor_copy(out=wb[j % 2][:], in_=wf[j % 2][:]), reads=[Bwf[j % 2]], writes=[Bwb[j % 2]])
                for g in range(4):
                    i2 = cnt % 2
                    cnt += 1
                    pb = i2
                    for k in range(16):
                        S.op('pe', lambda e: e.matmul(bank(pb), lhsT=wb[j % 2][:, k, :], rhs=h2T[:, k, g * 512:(g + 1) * 512],
                                                      start=(k == 0), stop=(k == 15)), reads=[Bwb[j % 2], Bh2T], writes=[PB[pb]])
                    S.op('act', lambda e: e.activation(out=qTs[i2][:], in_=bank(pb), func=AF.Copy), reads=[PB[pb]], writes=[BqTs[i2]])
                    for t4 in range(4):
                        S.op('pe', lambda e: e.matmul(bank(2 + i2)[:, t4 * 128:(t4 + 1) * 128], lhsT=qTs[i2][:, t4 * 128:(t4 + 1) * 128], rhs=skt[:, j, :],
                                                      start=True, stop=True), reads=[BqTs[i2], Bsk], writes=[PB[2 + i2]])
                    S.op('dve', lambda e: e.tensor_copy(out=ssb[i2][:], in_=bank(2 + i2)), reads=[PB[2 + i2]], writes=[Bssb[i2]])
                    dst = s_d[j, g * 512:(g + 1) * 512, :].rearrange("(a p) k -> p a k", p=128)
                    S.dma('act', dst, ssb[i2][:].rearrange("p (a k) -> p a k", k=128), reads=[Bssb[i2]], writes=[Bsd], owner=Bssb[i2], part=True)
            S.barrier()
        if dbg:
            with ExitStack() as phd:
                tmps = sbt(phd, "tmps", [128, 16, 128], F32); Bts = S.buf("tmps")
                for i in range(16):
                    S.dma('sp', tmps[:], s_d[:, i * 128:(i + 1) * 128, :].rearrange("j p k -> p j k"), reads=[Bsd], writes=[Bts])
                    S.dma('sp', dbg_out["d_s"][i * 128:(i + 1) * 128, :].rearrange("p (j k) -> p j k", k=128), tmps[:], reads=[Bts], writes=[S.buf("dbgs")], owner=Bts)
                S.barrier()
        if stop_after == "4a":
            return nc, S

        BGT = S.buf("GT_d")
        NSUB = 16
        with ExitStack() as ph:
            stm = [sbt(ph, "stm0", [128, 16, 128], F32)] * 2; Bstm = [S.buf("stm0")] * 2
            wk = sbt(ph, "wk", [128, 16, 128], F32); Bwk = S.buf("wk")
            top = sbt(ph, "top", [128, 16, 16], F32); Btop = S.buf("top")
            cand = sbt(ph, "cand", [128, 8, 16, 16], F32); Bcand = S.buf("cand")
            cwk = wk[:].rearrange("p (h two) k -> p h (two k)", two=2); Bcwk = Bwk
            best = sbt(ph, "best", [128, 8, 16], F32); Bbest = S.buf("best")
            eb = sbt(ph, "eb", [128, 8, 16], F32); Beb = S.buf("eb")
            Zs = sbt(ph, "Zs", [128, 8], F32); rZ = sbt(ph, "rZ", [128, 8], F32); BZ = S.buf("Z")
            tm4 = sbt(ph, "tm4", [128, 4, 8, 16], F32); Btm4 = S.buf("tm4")
            colT = [sbt(ph, f"colT{i}", [128, 4, 128], F32) for i in range(2)]; BcolT = S.bufs("colT", 2)
            S0 = [sbt(ph, f"S0r{i}", [128, NSUB, 128], F32) for i in range(2)]; BS0 = S.bufs("S0r", 2)
            S1 = [sbt(ph, f"S1r{i}", [128, NSUB, 128], F32) for i in range(2)]; BS1 = S.bufs("S1r", 2)
            Mks = [sbt(ph, f"Mk{i}", [128, NSUB, 128], BF16) for i in range(2)]; BMks = S.bufs("Mk", 2)
            D1s = [sbt(ph, f"D1{i}", [128, NSUB, 128], F32) for i in range(2)]; BD1s = S.bufs("D1", 2)
            E1s = [sbt(ph, f"E1{i}", [128, NSUB, 128], BF16) for i in range(2)]; BE1s = S.bufs("E1", 2)
            Rr = [sbt(ph, f"Rr{i}", [128, NSUB, 128], BF16) for i in range(2)]; BRr = S.bufs("Rr", 2)
            OH = [sbt(ph, f"OH{i}", [128, NSUB, 128], BF16) for i in range(2)]; BOH = S.bufs("OH", 2)
            GTs = [sbt(ph, "GTs0", [128, 128, 128], BF16)] * 2; BGTs = [S.buf("GTs0")] * 2
            nsub = 128 // NSUB
            uf = [sbt(ph, f"uf{i}", [128, D], F32) for i in range(2)]
            ub = [sbt(ph, f"ub{i}", [128, D], BF16) for i in range(2)]
            Buf_ = S.bufs("uf", 2); Bub = S.bufs("ub", 2)

            def load_tab(c):
                S.dma('sp', uf[c % 2][:], u_tab[c * 128:(c + 1) * 128, :], writes=[Buf_[c % 2]])

            def prep_tab(c):
                if c + 1 < 128:
                    load_tab(c + 1)
                i = c % 2
                for q4 in range(4):
                    pb = q4
                    for j in range(4):
                        k = q4 * 4 + j
                        S.op('pe', lambda e: e.transpose(out=bank(pb)[:, j * 128:(j + 1) * 128], in_=uf[i][:, k * 128:(k + 1) * 128], identity=idf[:]),
                             reads=[Buf_[i], Bc], writes=[PB[pb]])
                    S.op('act', lambda e: e.activation(out=ub[i][:, q4 * 512:(q4 + 1) * 512], in_=bank(pb), func=AF.Copy), reads=[PB[pb]], writes=[Bub[i]])
                S.dma('act', uT_d[c], ub[i][:], reads=[Bub[i]], writes=[BuT], owner=Bub[i], part=True)
            load_tab(0)

            def load_rep(ti, sb_i, slot):
                tok0 = ti * 128 + sb_i * NSUB
                for p, (dst, Bd) in enumerate(((S0[slot], BS0[slot]), (S1[slot], BS1[slot]))):
                    src = bass.AP(s_d.tensor, p * T * 128 + tok0 * 128, [[2 * T * 128, 8], [0, 16], [1, NSUB * 128]])
                    S.dma('sp', dst[:].rearrange("q t k -> q (t k)"), src, reads=[Bsd], writes=[Bd])

            S.dma('sp', stm[0][:], s_d[:, 0:128, :].rearrange("j p k -> p j k"), reads=[Bsd], writes=[Bstm[0]])
            gsub = 0
            for ti in range(16):
                st = stm[ti % 2]; Bst_ = Bstm[ti % 2]
                load_rep(ti, 0, 0)
                for j in range(16):
                    S.op('dve', lambda e: e.max(out=top[:, j, 0:8], in_=st[:, j, :]), reads=[Bst_], writes=[Btop])
                    S.op('dve', lambda e: e.match_replace(out=wk[:, j, :], in_to_replace=top[:, j, 0:8], in_values=st[:, j, :], imm_value=NEG),
                         reads=[Bst_, Btop], writes=[Bwk])
                    S.op('dve', lambda e: e.max(out=top[:, j, 8:16], in_=wk[:, j, :]), reads=[Bwk], writes=[Btop])
                if ti + 1 < 16:
                    S.dma('sp', stm[(ti + 1) % 2][:], s_d[:, (ti + 1) * 128:(ti + 2) * 128, :].rearrange("j p k -> p j k"), reads=[Bsd], writes=[Bstm[(ti + 1) % 2]])
                tv = top[:].rearrange("p (h two) a -> p h two a", two=2)
                S.op('dve', lambda e: e.tensor_tensor(out=cand[:], in0=tv[:, :, 0, :].unsqueeze(3).to_broadcast([128, 8, 16, 16]),
                                                      in1=tv[:, :, 1, :].unsqueeze(2).to_broadcast([128, 8, 16, 16]), op=ALU.add), reads=[Btop], writes=[Bcand])
                cv = cand[:].rearrange("p h a b -> p h (a b)")
                for h in range(8):
                    S.op('dve', lambda e: e.max(out=best[:, h, 0:8], in_=cv[:, h, :]), reads=[Bcand], writes=[Bbest])
                    S.op('dve', lambda e: e.match_replace(out=cwk[:, h, :], in_to_replace=best[:, h, 0:8], in_values=cv[:, h, :], imm_value=NEG),
                         reads=[Bcand, Bbest], writes=[Bcwk])
                    S.op('dve', lambda e: e.max(out=best[:, h, 8:16], in_=cwk[:, h, :]), reads=[Bcwk], writes=[Bbest])
                S.op('dve', lambda e: e.tensor_tensor(out=eb[:], in0=best[:], in1=best[:, :, 0:1].to_broadcast([128, 8, 16]), op=ALU.subtract), reads=[Bbest], writes=[Beb])
                S.op('act', lambda e: e.activation(out=eb[:], in_=eb[:], func=AF.Exp), reads=[Beb], writes=[Beb])
                S.op('dve', lambda e: e.tensor_reduce(out=Zs[:], in_=eb[:], axis=AX.X, op=ALU.add), reads=[Beb], writes=[BZ])
                S.op('dve', lambda e: e.tensor_copy(out=tm4[:, 0, :, :], in_=tv[:, :, 0, :]), reads=[Btop], writes=[Btm4])
                S.op('dve', lambda e: e.tensor_copy(out=tm4[:, 2, :, :], in_=best[:, :, 15:16].to_broadcast([128, 8, 16])), reads=[Bbest], writes=[Btm4])
                S.op('act', lambda e: e.activation(out=rZ[:], in_=Zs[:], func=AF.Ln), reads=[BZ], writes=[BZ])
                S.op('dve', lambda e: e.tensor_tensor(out=tm4[:, 3, :, :], in0=tv[:, :, 0, :], in1=best[:, :, 0:1].to_broadcast([128, 8, 16]), op=ALU.subtract),
                     reads=[Btop, Bbest], writes=[Btm4])
                S.op('dve', lambda e: e.tensor_tensor(out=tm4[:, 3, :, :], in0=tm4[:, 3, :, :], in1=rZ[:].unsqueeze(2).to_broadcast([128, 8, 16]), op=ALU.subtract),
                     reads=[Btm4, BZ], writes=[Btm4])
                cT = colT[ti % 2]; BcT = BcolT[ti % 2]
                for q in (0, 2, 3):
                    S.op('pe', lambda e: e.transpose(out=bank(7)[:, q * 128:(q + 1) * 128], in_=tm4[:, q, :, :].rearrange("p h a -> p (h a)"), identity=idf[:]),
                         reads=[Btm4, Bc], writes=[PB[7]])
                S.op('act', lambda e: e.activation(out=cT[:].rearrange("p q t -> p (q t)"), in_=bank(7), func=AF.Copy), reads=[PB[7]], writes=[BcT])
                GTt = GTs[ti % 2]; BGTt = BGTs[ti % 2]
                for c in range(ti * 8, ti * 8 + 8):
                    prep_tab(c)
                def stageA(sb_i):
                    slot = sb_i % 2
                    tl = sb_i * NSUB

                    def bc(qi):
                        return cT[:, qi, tl:tl + NSUB].unsqueeze(2).to_broadcast([128, NSUB, 128])
                    s0r = S0[slot]; s1r = S1[slot]
                    D1 = D1s[slot]; BD1 = BD1s[slot]; E1 = E1s[slot]; BE1 = BE1s[slot]; Mk = Mks[slot]; BMk = BMks[slot]
                    S.op('pool', lambda e: e.tensor_tensor(out=D1[:], in0=s1r[:], in1=bc(3), op=ALU.add), reads=[BS1[slot], BcT], writes=[BD1])
                    S.op('act', lambda e: e.activation(out=E1[:], in_=D1[:], func=AF.Exp), reads=[BD1], writes=[BE1])
                    S.op('dve', lambda e: e.tensor_tensor(out=s1r[:], in0=s1r[:], in1=bc(0), op=ALU.add), reads=[BS1[slot], BcT], writes=[BS1[slot]])
                    S.op('dve', lambda e: e.tensor_tensor(out=Mk[:], in0=s1r[:], in1=bc(2), op=ALU.is_ge), reads=[BS1[slot], BcT], writes=[BMk])
                    S.op('dve', lambda e: e.tensor_tensor(out=OH[slot][:], in0=s0r[:], in1=bc(0), op=ALU.is_equal), reads=[BS0[slot], BcT], writes=[BOH[slot]])
                    S.op('pool', lambda e: e.tensor_tensor(out=Rr[slot][:], in0=Mk[:], in1=E1[:], op=ALU.mult), reads=[BMk, BE1], writes=[BRr[slot]])

                def stageB(sb_i):
                    slot = sb_i % 2
                    tl = sb_i * NSUB
                    for t4 in range(NSUB // 4):
                        pb = 5 + (t4 % 2)
                        for tt in range(4):
                            t = t4 * 4 + tt
                            S.op('pe', lambda e: e.matmul(bank(pb)[:, tt * 128:(tt + 1) * 128], lhsT=Rr[slot][:, t, :], rhs=OH[slot][:, t, :], start=True, stop=True),
                                 reads=[BRr[slot], BOH[slot]], writes=[PB[pb]])
                        dst = GTt[:, :, tl + t4 * 4:tl + t4 * 4 + 4]
                        src = bank(pb).rearrange("p (t c) -> p c t", c=128)
                        S.op('act', lambda e: e.activation(out=dst, in_=src, func=AF.Copy), reads=[PB[pb]], writes=[BGTt])

                load_rep(ti, 1, 1)
                stageA(0)
                for sb_i in range(nsub):
                    if sb_i + 2 < nsub:
                        load_rep(ti, sb_i + 2, sb_i % 2)
                    if sb_i + 1 < nsub:
                        stageA(sb_i + 1)
                    stageB(sb_i)
                for c8 in range(8):
                    dst = GT_d[c8 * 16:(c8 + 1) * 16, :, ti * 128:(ti + 1) * 128].rearrange("c p t -> p c t")
                    S.dma('act', dst, GTt[:, c8 * 16:(c8 + 1) * 16, :], reads=[BGTt], writes=[BGT], owner=BGTt, part=True, maxfly=2)
            S.barrier()
        if dbg:
            with ExitStack() as phd:
                tmpg = sbt(phd, "tmpg", [128, 8, T], BF16); Btg = S.buf("tmpg")
                for i in range(16):
                    S.dma('sp', tmpg[:], GT_d[i * 8:(i + 1) * 8].rearrange("c p t -> p c t"), reads=[BGT], writes=[Btg])
                    S.dma('sp', dbg_out["d_GT"][i * 8:(i + 1) * 8].rearrange("c p t -> p c t"), tmpg[:], reads=[Btg], writes=[S.buf("dbgg")], owner=Btg)
                S.barrier()
        if stop_after == "4b":
            return nc, S

        Bout = S.buf("out")
        SC = 4
        NSC = 128 // SC
        with ExitStack() as ph:
            h2g = sbt(ph, "h2g50", [128, 16, 512], BF16); Bhg = S.buf("h2g50")
            acc = sbt(ph, "acc", [128, 4, D], F32); Bacc = S.buf("acc")
            uTc = [sbt(ph, f"uTc{i}", [128, D], BF16) for i in range(3)]; BuTc = S.bufs("uTc", 3)
            vcs = [sbt(ph, f"vcs{i}", [128, SC, D], BF16) for i in range(2)]; Bvcs = S.bufs("vcs", 2)
            gtc = [sbt(ph, f"gtc{i}", [128, 512], BF16) for i in range(3)]; Bgtc = S.bufs("gtc", 3)
            gl = [sbt(ph, f"gl{i}", [128, 512], BF16) for i in range(2)]; Bgl = S.bufs("gl", 2)
            AG = [sbt(ph, f"AG{i}", [128, SC, 512], BF16) for i in range(2)]; BAG = S.bufs("AG", 2)
            x1t = sbt(ph, "x1f0", [128, D], F32); Bx1t = S.buf("x1f0")
            ot = [sbt(ph, f"ot{i}", [128, D], F32) for i in range(2)]; Bot = S.bufs("ot", 2)

            vfs = [sbt(ph, f"vfs{i}", [128, D], F32) for i in range(4)]; Bvfs = S.bufs("vfs", 4)

            def load_chunk(g, c):
                i3 = c % 3
                S.dma('sp', uTc[i3][:], uT_d[c], reads=[BuT], writes=[BuTc[i3]])
                S.dma('sp', gtc[i3][:], GT_d[c, :, g * 512:(g + 1) * 512], reads=[BGT], writes=[Bgtc[i3]])
                S.dma('sp', vfs[c % 4][:], v_tab[c * 128:(c + 1) * 128, :], writes=[Bvfs[c % 4]])

            def load_v(sc):
                pass

            def first_phase(g, sc):
                ag = AG[sc % 2]; Bag = BAG[sc % 2]
                for cl in range(SC):
                    c = sc * SC + cl
                    if c + 2 < 128:
                        load_chunk(g, c + 2)
                    i3 = c % 3
                    pb = c % 2
                    for k in range(16):
                        S.op('pe', lambda e: e.matmul(bank(pb), lhsT=uTc[i3][:, k * 128:(k + 1) * 128], rhs=h2g[:, k, :], start=(k == 0), stop=(k == 15)),
                             reads=[BuTc[i3], Bhg], writes=[PB[pb]])
                    S.op('act', lambda e: e.activation(out=gl[pb][:], in_=bank(pb), func=AF.Gelu), reads=[PB[pb]], writes=[Bgl[pb]])
                    S.op('pool', lambda e: e.tensor_tensor(out=ag[:, cl, :], in0=gl[pb][:], in1=gtc[i3][:], op=ALU.mult), reads=[Bgl[pb], Bgtc[i3]], writes=[Bag])
                    S.op('act', lambda e: e.activation(out=vcs[sc % 2][:, cl, :], in_=vfs[c % 4][:], func=AF.Copy), reads=[Bvfs[c % 4]], writes=[Bvcs[sc % 2]])

            def second_phase(sc):
                ag = AG[sc % 2]; Bag = BAG[sc % 2]
                vv = vcs[sc % 2]; Bvv = Bvcs[sc % 2]
                for tt in range(4):
                    for db in range(4):
                        pb = 2 + (tt * 4 + db) % 6
                        for cl in range(SC):
                            S.op('pe', lambda e: e.matmul(bank(pb), lhsT=ag[:, cl, tt * 128:(tt + 1) * 128], rhs=vv[:, cl, db * 512:(db + 1) * 512],
                                                          start=(cl == 0), stop=(cl == SC - 1)), reads=[Bag, Bvv], writes=[PB[pb]])
                        a_ap = acc[:, tt, db * 512:(db + 1) * 512]
                        if sc == 0:
                            S.op('dve', lambda e: e.tensor_copy(out=a_ap, in_=bank(pb)), reads=[PB[pb]], writes=[Bacc])
                        else:
                            S.op('dve', lambda e: e.tensor_tensor(out=a_ap, in0=bank(pb), in1=a_ap, op=ALU.add), reads=[PB[pb], Bacc], writes=[Bacc])

            for g in range(4):
                S.dma('sp', h2g[:], h2_d[:, :, g * 512:(g + 1) * 512], reads=[Bh2], writes=[Bhg])
                load_chunk(g, 0)
                load_chunk(g, 1)
                load_v(0)
                for sc in range(NSC):
                    first_phase(g, sc)
                    if sc > 0:
                        second_phase(sc - 1)
                    if sc + 1 < NSC:
                        load_v(sc + 1)
                second_phase(NSC - 1)
                for tt in range(4):
                    ti = g * 4 + tt
                    i2 = ti % 2
                    S.dma('sp', x1t[:], x1_d[ti * 128:(ti + 1) * 128, :], reads=[Bx1], writes=[Bx1t])
                    S.op('dve', lambda e: e.tensor_tensor(out=ot[i2][:], in0=acc[:, tt, :], in1=gt2bc[:], op=ALU.mult), reads=[Bacc, Bgtbc], writes=[Bot[i2]])
                    S.op('pool', lambda e: e.tensor_tensor(out=ot[i2][:], in0=ot[i2][:], in1=x1t[:], op=ALU.add), reads=[Bot[i2], Bx1t], writes=[Bot[i2]])
                    S.dma('act', out[ti * 128:(ti + 1) * 128, :], ot[i2][:], reads=[Bot[i2]], writes=[Bout], owner=Bot[i2], part=True)
            S.barrier()
    return nc, S


_CACHE = {}


def kernel(**inputs):
    inp = {k: np.asarray(v) for k, v in inputs.items()}
    if "nc" not in _CACHE:
        _CACHE["nc"] = build_program()[0]
    nc = _CACHE["nc"]
    in_maps = [_host_inputs(inp, b) for b in range(8)]
    res = run_bass_kernel_spmd(nc, in_maps, core_ids=list(range(8)))
    return np.stack([np.asarray(r["out"], np.float32) for r in res.results], axis=0)
```

```python
import os
from contextlib import ExitStack
import numpy as np
import concourse.bass as bass
import concourse.mybir as mybir
from concourse.bass_utils import run_bass_kernel_spmd

F32 = mybir.dt.float32
BF16 = mybir.dt.bfloat16
AF = mybir.ActivationFunctionType
ALU = mybir.AluOpType
AX = mybir.AxisListType

D = 2048
T = 2048
L = 256
TT = T + L
EPS = 1e-6
NTYPE = 21
NEG = -1e30


class Buf:
    __slots__ = ("name", "w", "r", "dsem", "dcnt", "ro")

    def __init__(self, name):
        self.name = name
        self.w = None
        self.r = []
        self.dsem = None
        self.dcnt = 0
        self.ro = False


class Sched:
    def __init__(self, nc, es):
        self.nc = nc
        self.es = es
        self.eng = {'pe': nc.tensor, 'dve': nc.vector, 'act': nc.scalar, 'pool': nc.gpsimd, 'sp': nc.sync}
        self.sem = {e: es.enter_context(nc.semaphore('s_' + e)) for e in ['pe', 'dve', 'act', 'pool']}
        self.cnt = {e: 0 for e in self.sem}
        self.seen = {e: {} for e in self.eng}
        self.dsems = []
        self.ninst = 0

    def buf(self, name):
        return Buf(name)

    def bufs(self, name, n):
        return [Buf(f"{name}{i}") for i in range(n)]

    def _wait(self, e, dep):
        kind, key, val = dep
        if kind == 'eng':
            if key == e and e == 'pe':
                return
            sem = self.sem[key]
            k = key
        else:
            sem = key[0]
            k = id(key[0])
        if self.seen[e].get(k, 0) >= val:
            return
        self.eng[e].wait_ge(sem, val)
        self.seen[e][k] = val

    def _deps(self, e, reads, writes, part=False):
        for b in reads:
            if b.w is not None:
                self._wait(e, b.w)
        for b in writes:
            if b.w is not None and not part:
                self._wait(e, b.w)
            for d in b.r:
                self._wait(e, d)

    def _mark(self, me, reads, writes):
        for b in reads:
            if not b.ro:
                b.r.append(me)
                if len(b.r) > 64:
                    last = {}
                    for d in b.r:
                        kk = d[1] if d[0] == 'eng' else d[1][1]
                        last[(d[0], kk)] = d
                    b.r = list(last.values())
        for b in writes:
            b.w = me
            b.r = []

    def op(self, e, fn, reads=(), writes=()):
        self._deps(e, reads, writes)
        ins = fn(self.eng[e])
        self.cnt[e] += 1
        self.ninst += 1
        ins.then_inc(self.sem[e], 1)
        self._mark(('eng', e, self.cnt[e]), reads, writes)
        return ins

    def dma(self, q, out, in_, reads=(), writes=(), owner=None, part=False, maxfly=None, **kw):
        self._deps(q, reads, writes, part)
        if owner is None:
            owner = writes[0] if writes else reads[0]
        if owner.dsem is None:
            owner.dsem = self.es.enter_context(self.nc.semaphore('d_' + owner.name))
            self.dsems.append(owner)
        if maxfly is not None and owner.dcnt >= 16 * maxfly:
            self._wait(q, ('dma', (owner.dsem, 'd_' + owner.name), owner.dcnt - 16 * (maxfly - 1)))
        ins = self.eng[q].dma_start(out=out, in_=in_, **kw)
        owner.dcnt += 16
        self.ninst += 1
        ins.then_inc(owner.dsem, 16)
        self._mark(('dma', (owner.dsem, 'd_' + owner.name), owner.dcnt), reads, writes)
        return ins

    def barrier(self, engines=('pe', 'dve', 'act', 'pool', 'sp')):
        for e in engines:
            for k in self.sem:
                if self.cnt[k] > 0:
                    self._wait(e, ('eng', k, self.cnt[k]))
            for o in self.dsems:
                if o.dcnt > 0:
                    self._wait(e, ('dma', (o.dsem, 'd_' + o.name), o.dcnt))


def _attn_types():
    types = [(8, 8 + d) for d in (-2, -1, 0, 1, 2)]
    for qb in (0, 1):
        types += [(qb, kt) for kt in range(4)]
    for qb in (14, 15):
        types += [(qb, kt) for kt in range(12, 16)]
    return types


def _qb_plan(qb):
    if 2 <= qb <= 13:
        return 0, [qb + d for d in (-2, -1, 0, 1, 2)]
    ei = {0: 0, 1: 1, 14: 2, 15: 3}[qb]
    kts = list(range(4)) if qb < 2 else list(range(12, 16))
    return 5 + 4 * ei, kts


def _bias_tables(rpb):
    types = _attn_types()
    key = np.arange(128)
    q = np.arange(128)
    rkl, kc = key // 64, key % 64
    rl, qc = q // 64, q % 64
    biasT = np.zeros((8, 128, NTYPE, 128), np.float32)
    maskT = np.zeros((128, NTYPE, 128), np.float32)
    for ti, (qb, kt) in enumerate(types):
        r = 2 * qb + rl
        rk = 2 * kt + rkl
        rs = np.clip(r - 4, 0, 24)
        rowv = (rk[:, None] >= rs[None, :]) & (rk[:, None] < rs[None, :] + 8)
        cs = np.clip(qc - 8, 0, 48)
        colv = (kc[:, None] >= cs[None, :]) & (kc[:, None] < cs[None, :] + 16)
        dr = np.clip(rk[:, None] - r[None, :] + 7, 0, 14)
        dc = np.clip(kc[:, None] - qc[None, :], -15, 15) + 15
        biasT[:, :, ti, :] = rpb[:, dr, dc]
        maskT[:, ti, :] = (rowv & colv).astype(np.float32)
    return biasT, maskT


G_G1, G_G2, G_QN, G_KN, G_GATT, G_GCONV, G_WC, G_N = 0, 16, 32, 33, 34, 42, 50, 74


def _host_inputs(inp, b):
    f = np.float32
    m = {}
    m["x"] = np.ascontiguousarray(inp["x"][b], f)
    m["ctx"] = np.ascontiguousarray(inp["ctx"][b], f)
    cc = np.stack([inp["c"][b].reshape(16, 128).T, inp["c_ctx"].reshape(16, 128).T], axis=-1)
    m["cc"] = np.ascontiguousarray(cc, f)
    m["w_ada"] = np.ascontiguousarray(inp["w_ada"][0], f)
    m["b_adaT"] = np.ascontiguousarray(inp["b_ada"][0].reshape(96, 128).T, f)
    gv = np.zeros((128, G_N), f)
    gv[:, G_G1:G_G1 + 16] = inp["g_norm1"][0].reshape(16, 128).T
    gv[:, G_G2:G_G2 + 16] = inp["g_norm2"][0].reshape(16, 128).T
    gv[:, G_QN] = inp["q_norm"][0]
    gv[:, G_KN] = inp["k_norm"][0]
    gv[:, G_GATT:G_GATT + 8] = inp["g_attn_out"][0].T
    gv[:, G_GCONV:G_GCONV + 8] = inp["g_conv_out"][0].T
    gv[:, G_WC:G_WC + 24] = inp["w_conv"][0].reshape(3, 8, 128).transpose(2, 1, 0).reshape(128, 24)
    m["gvec"] = gv
    m["w_in"] = np.ascontiguousarray(inp["w_in"][0], f)
    m["w_out"] = np.ascontiguousarray(inp["w_out"][0], f)
    m["w_pq"] = np.ascontiguousarray(inp["w_pq"][0], f)
    m["skT"] = np.ascontiguousarray(inp["sub_keys"][0].reshape(16, 128, 128).transpose(2, 0, 1), f)
    m["u_tab"] = np.ascontiguousarray(inp["u_experts"][0], f)
    m["v_tab"] = np.ascontiguousarray(inp["v_experts"][0], f)
    biasT, maskT = _bias_tables(np.asarray(inp["rpb"][0], f))
    m["biasT"] = biasT
    m["maskT"] = maskT
    m["ident"] = np.eye(128, dtype=f)
    return m


def build_program(stop_after=None, dbg=False):
    nc = bass.Bass("TRN2", target_bir_lowering=False)

    def din(name, shape, dt=F32):
        return nc.dram_tensor(name, list(shape), dt, kind="ExternalInput").ap()

    def dscr(name, shape, dt):
        return nc.dram_tensor(name, list(shape), dt, kind="Internal").ap()

    x = din("x", [T, D]); ctx = din("ctx", [L, D]); cc = din("cc", [128, 16, 2])
    w_ada = din("w_ada", [D, 6 * D]); b_adaT = din("b_adaT", [128, 96]); gvec = din("gvec", [128, G_N])
    w_in = din("w_in", [D, 6144]); w_out = din("w_out", [D, D]); w_pq = din("w_pq", [D, D])
    skT = din("skT", [128, 16, 128]); u_tab = din("u_tab", [16384, D]); v_tab = din("v_tab", [16384, D])
    biasT = din("biasT", [8, 128, NTYPE, 128]); maskT = din("maskT", [128, NTYPE, 128]); ident = din("ident", [128, 128])
    out = nc.dram_tensor("out", [T, D], F32, kind="ExternalOutput").ap()

    uT_d = dscr("uT_d", [128, 128, D], BF16)
    cat_d = dscr("cat_d", [16, 128, T], BF16)
    x1_d = dscr("x1_d", [T, D], F32)
    h2_d = dscr("h2_d", [128, 16, T], BF16)
    s_d = dscr("s_d", [16, T, 128], F32)
    GT_d = dscr("GT_d", [128, 128, T], BF16)
    gt_d = dscr("gt_d", [32, 128], F32)
    dbg_out = None
    if dbg:
        dbg_out = {
            "d_mod": nc.dram_tensor("d_mod", [128, 96, 2], F32, kind="ExternalOutput").ap(),
            "d_hT": nc.dram_tensor("d_hT", [128, 16, TT], BF16, kind="ExternalOutput").ap(),
            "d_cat": nc.dram_tensor("d_cat", [16, 128, T], BF16, kind="ExternalOutput").ap(),
            "d_x1": nc.dram_tensor("d_x1", [T, D], F32, kind="ExternalOutput").ap(),
            "d_h2": nc.dram_tensor("d_h2", [128, 16, T], BF16, kind="ExternalOutput").ap(),
            "d_s": nc.dram_tensor("d_s", [T, 2048], F32, kind="ExternalOutput").ap(),
            "d_GT": nc.dram_tensor("d_GT", [128, 128, T], BF16, kind="ExternalOutput").ap(),
        }

    with ExitStack() as es:
        S = Sched(nc, es)
        PS = es.enter_context(nc.psum_tensor("PS", [128, 4096], F32))
        PB = S.bufs("psb", 8)

        def bank(i, n=512):
            return PS[:, i * 512:i * 512 + n]

        def sbt(stack, name, shape, dt):
            return stack.enter_context(nc.sbuf_tensor(name, list(shape), dt))

        idf = sbt(es, "idf", [128, 128], F32); idb = sbt(es, "idb", [128, 128], BF16)
        onesb = sbt(es, "onesb", [128, 128], BF16)
        gv = sbt(es, "gv", [128, G_N], F32)
        modT = sbt(es, "modT", [128, 96, 2], F32)
        der = sbt(es, "der", [128, 6, 16], F32)
        gt12 = sbt(es, "gt12", [128, 32], F32)
        epsb = sbt(es, "epsb", [128, 1], F32)
        Bc = S.buf("consts"); Bmod = S.buf("modT"); Bder = S.buf("der"); Bgtbc = S.buf("gtbc")

        S.dma('sp', idf[:], ident, writes=[Bc])
        S.dma('sp', gv[:], gvec, writes=[Bc])
        S.op('dve', lambda e: e.tensor_copy(out=idb[:], in_=idf[:]), reads=[Bc], writes=[Bc])
        S.op('dve', lambda e: e.memset(onesb[:], 1.0), writes=[Bc])
        S.op('dve', lambda e: e.memset(epsb[:], EPS), writes=[Bc])

        def rstd_from_ss(ss_ap, out_ap, tmp_ap, n_inv, reads, writes, tmpbuf):
            S.op('act', lambda e: e.activation(out=tmp_ap, in_=ss_ap, func=AF.Sqrt, scale=n_inv, bias=epsb[:ss_ap.shape[0], 0:1]),
                 reads=list(reads) + [Bc], writes=[tmpbuf])
            S.op('dve', lambda e: e.reciprocal(out=out_ap, in_=tmp_ap), reads=[tmpbuf], writes=writes)

        BuT = S.buf("uT_d")

        def make_prep(ph, banks, tag):
            uf = [sbt(ph, f"uf{tag}{i}", [128, D], F32) for i in range(2)]
            ub = [sbt(ph, f"ub{tag}{i}", [128, D], BF16) for i in range(2)]
            Buf_ = S.bufs("uf" + tag, 2); Bub = S.bufs("ub" + tag, 2)

            def load_tab(c):
                S.dma('sp', uf[c % 2][:], u_tab[c * 128:(c + 1) * 128, :], writes=[Buf_[c % 2]])

            def prep_tab(c, last):
                if c + 1 < last:
                    load_tab(c + 1)
                i = c % 2
                for q4 in range(4):
                    pb = banks[q4 % len(banks)]
                    for j in range(4):
                        k = q4 * 4 + j
                        S.op('pe', lambda e: e.transpose(out=bank(pb)[:, j * 128:(j + 1) * 128], in_=uf[i][:, k * 128:(k + 1) * 128], identity=idf[:]),
                             reads=[Buf_[i], Bc], writes=[PB[pb]])
                    S.op('act', lambda e: e.activation(out=ub[i][:, q4 * 512:(q4 + 1) * 512], in_=bank(pb), func=AF.Copy), reads=[PB[pb]], writes=[Bub[i]])
                S.dma('act', uT_d[c], ub[i][:], reads=[Bub[i]], writes=[BuT], owner=Bub[i], part=True)
            return load_tab, prep_tab
        if stop_after == "T":
            return nc, S

        with ExitStack() as ph:
            ccs = sbt(ph, "ccs", [128, 16, 2], F32)
            ccr = sbt(ph, "ccr", [128, 16, 2], F32)
            bad = sbt(ph, "bad", [128, 96], F32)
            wa = [sbt(ph, f"wa{i}", [128, 16, 512], F32) for i in range(2)]
            Bwa = S.bufs("wa", 2); Bcc = S.buf("cc")
            S.dma('sp', ccr[:], cc, writes=[Bcc])
            S.dma('sp', bad[:], b_adaT, writes=[Bcc])
            S.op('act', lambda e: e.activation(out=ccs[:], in_=ccr[:], func=AF.Silu), reads=[Bcc], writes=[Bcc])
            w_ada_v = w_ada.rearrange("(k p) f -> p k f", p=128)

            def load_wa(fb):
                S.dma('sp', wa[fb % 2][:], w_ada_v[:, :, fb * 512:(fb + 1) * 512], writes=[Bwa[fb % 2]])
            load_wa(0)
            mps = bank(0, 192).rearrange("p (j two) -> p j two", two=2)
            for fb in range(24):
                if fb + 1 < 24:
                    load_wa(fb + 1)
                for m in range(4):
                    j = fb * 4 + m
                    for k in range(16):
                        S.op('pe', lambda e: e.matmul(mps[:, j, :], lhsT=wa[fb % 2][:, k, m * 128:(m + 1) * 128], rhs=ccs[:, k, :],
                                                      start=(k == 0), stop=(k == 15)),
                             reads=[Bwa[fb % 2], Bcc], writes=[PB[0]])
            S.op('dve', lambda e: e.tensor_tensor(out=modT[:], in0=mps, in1=bad[:].unsqueeze(2).to_broadcast([128, 96, 2]), op=ALU.add),
                 reads=[PB[0], Bcc], writes=[Bmod])
            g1 = gv[:, G_G1:G_G1 + 16]; g2 = gv[:, G_G2:G_G2 + 16]
            for (dst, sc_m, col, g) in ((0, 1, 0, g1), (2, 1, 1, g1), (4, 4, 0, g2)):
                S.op('dve', lambda e: e.scalar_tensor_tensor(out=der[:, dst, :], in0=modT[:, sc_m * 16:(sc_m + 1) * 16, col], scalar=1.0, in1=g,
                                                             op0=ALU.add, op1=ALU.mult), reads=[Bmod, Bc], writes=[Bder])
            for (dst, sh_m, col) in ((1, 0, 0), (3, 0, 1), (5, 3, 0)):
                S.op('dve', lambda e: e.tensor_copy(out=der[:, dst, :], in_=modT[:, sh_m * 16:(sh_m + 1) * 16, col]), reads=[Bmod], writes=[Bder])
            S.op('dve', lambda e: e.tensor_copy(out=gt12[:, 0:16], in_=modT[:, 32:48, 0]), reads=[Bmod], writes=[Bder])
            S.op('dve', lambda e: e.tensor_copy(out=gt12[:, 16:32], in_=modT[:, 80:96, 0]), reads=[Bmod], writes=[Bder])
            if dbg:
                S.dma('sp', dbg_out["d_mod"], modT[:], reads=[Bmod], writes=[S.buf("dbgm")])
            S.barrier()
        if stop_after == "0":
            return nc, S

        A1x, sh1x, A1c, sh1c, A2, sh2 = [der[:, i, :] for i in range(6)]

        def norm_transpose(ph_bufs, xt_ap, Bxt, A_ap, sh_ap, dst_fn, Bdst, pbanks, extra_reads=()):
            junk, Bjunk, ssq, rt, rstd, Bst, xs, Bxs = ph_bufs
            S.op('act', lambda e: e.activation(out=junk[:], in_=xt_ap, func=AF.Square, accum_out=ssq[:]), reads=[Bxt], writes=[Bjunk, Bst])
            rstd_from_ss(ssq[:], rstd[:], rt[:], 1.0 / D, [Bst], [Bst], Bst)
            S.op('dve', lambda e: e.tensor_scalar(out=xs[:], in0=xt_ap, scalar1=rstd[:, 0:1], scalar2=None, op0=ALU.mult), reads=[Bxt, Bst], writes=[Bxs])
            for half in range(2):
                pb = pbanks[half]
                psb = bank(pb).bitcast(BF16)
                for j in range(8):
                    k = half * 8 + j
                    S.op('pe', lambda e: e.transpose(out=psb[:, j * 128:(j + 1) * 128], in_=xs[:, k * 128:(k + 1) * 128], identity=idb[:]),
                         reads=[Bxs, Bc], writes=[PB[pb]])
                for j in range(8):
                    k = half * 8 + j
                    if j % 2 == 0:
                        S.op('act', lambda e: e.activation(out=dst_fn(k), in_=psb[:, j * 128:(j + 1) * 128], func=AF.Identity,
                                                           scale=A_ap[:, k:k + 1], bias=sh_ap[:, k:k + 1]),
                             reads=[PB[pb], Bder] + list(extra_reads), writes=[Bdst])
                    else:
                        S.op('dve', lambda e: e.tensor_scalar(out=dst_fn(k), in0=psb[:, j * 128:(j + 1) * 128], scalar1=A_ap[:, k:k + 1],
                                                              scalar2=sh_ap[:, k:k + 1], op0=ALU.mult, op1=ALU.add),
                             reads=[PB[pb], Bder] + list(extra_reads), writes=[Bdst])

        def make_nt_bufs(ph, tag):
            junk = sbt(ph, "junk" + tag, [128, D], BF16); ssq = sbt(ph, "ssq" + tag, [128, 1], F32)
            rt = sbt(ph, "rt" + tag, [128, 1], F32); rstd = sbt(ph, "rstd" + tag, [128, 1], F32)
            xs = sbt(ph, "xs" + tag, [128, D], BF16)
            return (junk, S.buf("junk" + tag), ssq, rt, rstd, S.buf("st" + tag), xs, S.buf("xs" + tag))

        Bcat = S.buf("cat_d")
        with ExitStack() as ph:
            hT = sbt(ph, "hT", [128, 16, TT], BF16)
            BhT = S.buf("hT")
            with ExitStack() as p1a:
                xt = [sbt(p1a, f"xt{i}", [128, D], F32) for i in range(2)]
                Bxt = S.bufs("xt", 2)
                ntb = make_nt_bufs(p1a, "a")

                def load_x(i):
                    src = x[i * 128:(i + 1) * 128, :] if i < 16 else ctx[(i - 16) * 128:(i - 15) * 128, :]
                    S.dma('sp', xt[i % 2][:], src, writes=[Bxt[i % 2]])
                load_x(0)
                for i in range(18):
                    if i + 1 < 18:
                        load_x(i + 1)
                    A_ap, sh_ap = (A1x, sh1x) if i < 16 else (A1c, sh1c)
                    norm_transpose(ntb, xt[i % 2][:], Bxt[i % 2], A_ap, sh_ap,
                                   lambda k: hT[:, k, i * 128:(i + 1) * 128], BhT, (0, 1))
                S.barrier()
            if dbg:
                S.dma('sp', dbg_out["d_hT"], hT[:], reads=[BhT], writes=[S.buf("dbgh")])
            BhT.ro = True

            w_in_v = w_in.rearrange("(k p) f -> p k f", p=128)
            wf = [sbt(ph, f"wf{i}", [128, 16, 128], F32) for i in range(3)]
            wb = [sbt(ph, f"wb{i}", [128, 16, 128], BF16) for i in range(3)]
            Bwf = S.bufs("wf", 3); Bwb = S.bufs("wb", 3)
            sqb = sbt(ph, "sqb", [128, 512], BF16); Bsq = S.buf("sqb")
            rtt = sbt(ph, "rtt", [128, 512], F32); Brtt = S.buf("rtt")
            rsd = sbt(ph, "rsd", [128, 512], F32); Brsd = S.buf("rsd")
            catb = [sbt(ph, f"catb{i}", [128, T], BF16) for i in range(2)]
            Bcatb = S.bufs("catb", 2)

            def load_w(slot, col0):
                S.dma('sp', wf[slot][:], w_in_v[:, :, col0:col0 + 128], writes=[Bwf[slot]])

            def cast_w(slot, eng):
                if eng == 'act':
                    S.op('act', lambda e: e.activation(out=wb[slot][:], in_=wf[slot][:], func=AF.Copy), reads=[Bwf[slot]], writes=[Bwb[slot]])
                else:
                    S.op(eng, lambda e: e.tensor_copy(out=wb[slot][:], in_=wf[slot][:]), reads=[Bwf[slot]], writes=[Bwb[slot]])

            def group_norm_T(ps_ap, n, gcol_ap, dst_ap, Bdst, src_reads, pb_ss):
                S.op('act', lambda e: e.activation(out=sqb[:, :n], in_=ps_ap, func=AF.Square), reads=src_reads, writes=[Bsq])
                S.op('pe', lambda e: e.matmul(bank(pb_ss, n), lhsT=onesb[:], rhs=sqb[:, :n], start=True, stop=True), reads=[Bsq, Bc], writes=[PB[pb_ss]])
                rstd_from_ss(bank(pb_ss, n), rsd[:, :n], rtt[:, :n], 1.0 / 128, [PB[pb_ss]], [Brsd], Brtt)
                S.op('dve', lambda e: e.scalar_tensor_tensor(out=dst_ap, in0=ps_ap, scalar=gcol_ap, in1=rsd[:, :n], op0=ALU.mult, op1=ALU.mult),
                     reads=list(src_reads) + [Brsd, Bc], writes=[Bdst])

            with ExitStack() as p1b:
                QT = sbt(p1b, "QT", [128, T], BF16); KT = sbt(p1b, "KT", [128, TT], BF16)
                Vh = sbt(p1b, "Vh", [128, 18, 128], BF16)
                BQT = S.buf("QT"); BKT = S.buf("KT"); BVh = S.buf("Vh")
                bia = sbt(p1b, "bia", [128, NTYPE, 128], F32); msk = sbt(p1b, "msk", [128, NTYPE, 128], F32)
                Eh = sbt(p1b, "Eh", [128, NTYPE, 128], BF16)
                Bbia = S.buf("bia"); Bmsk = S.buf("msk"); BEh = S.buf("Eh")
                pa_es = [sbt(p1b, f"pa_e{i}", [128, 512], F32) for i in range(2)]; Bpaes = S.bufs("pae", 2)
                pb_es = [sbt(p1b, f"pb_e{i}", [128, 128], F32) for i in range(2)]; Bpbes = S.bufs("pbe", 2)
                PA = [sbt(p1b, f"PA{i}", [128, 512], BF16) for i in range(2)]
                PBt = [sbt(p1b, f"PBt{i}", [128, 384], BF16) for i in range(2)]
                BPA = S.bufs("PA", 2); BPBt = S.bufs("PBt", 2)
                rden = sbt(p1b, "rden", [128, 512], F32); Brden = S.buf("rden")
                osb = sbt(p1b, "osb", [128, 512], F32); Bosb = S.buf("osb")
                oraw = sbt(p1b, "oraw", [128, 512], F32); Boraw = S.buf("oraw")
                S.dma('sp', msk[:], maskT, writes=[Bmsk])
                scale = 128.0 ** -0.5
                def head_prologue(h):
                    load_w(0, 128 * h); load_w(1, 1024 + 128 * h); load_w(2, 2048 + 128 * h)
                    S.dma('sp', bia[:], biasT[h], writes=[Bbia])
                    cast_w(0, 'pool'); cast_w(1, 'pool'); cast_w(2, 'pool')
                    S.op('act', lambda e: e.activation(out=bia[:], in_=bia[:], func=AF.Exp), reads=[Bbia], writes=[Bbia])
                head_prologue(0)
                for h in range(8):
                    S.op('dve', lambda e: e.tensor_tensor(out=Eh[:], in0=bia[:], in1=msk[:], op=ALU.mult), reads=[Bbia, Bmsk], writes=[BEh])
                    for (slot, dst, Bd, ngrp, gcol) in ((0, QT, BQT, 4, G_QN), (1, KT, BKT, 5, G_KN)):
                        for g in range(ngrp):
                            n = 512 if g < 4 else 256
                            pb = g % 2
                            for k in range(16):
                                S.op('pe', lambda e: e.matmul(bank(pb, n), lhsT=wb[slot][:, k, :], rhs=hT[:, k, g * 512:g * 512 + n],
                                                              start=(k == 0), stop=(k == 15)), reads=[Bwb[slot], BhT], writes=[PB[pb]])
                            group_norm_T(bank(pb, n), n, gv[:, gcol:gcol + 1], dst[:, g * 512:g * 512 + n], Bd, [PB[pb]], 2)
                    for g in range(5):
                        pb = g % 2
                        nt = 4 if g < 4 else 2
                        for j in range(nt):
                            ti = g * 4 + j
                            for k in range(16):
                                S.op('pe', lambda e: e.matmul(bank(pb)[:, j * 128:(j + 1) * 128], lhsT=hT[:, k, ti * 128:(ti + 1) * 128], rhs=wb[2][:, k, :],
                                                              start=(k == 0), stop=(k == 15)), reads=[Bwb[2], BhT], writes=[PB[pb]])
                        S.op('act', lambda e: e.activation(out=Vh[:, g * 4:g * 4 + nt, :].rearrange("p a b -> p (a b)"), in_=bank(pb, nt * 128), func=AF.Copy),
                             reads=[PB[pb]], writes=[BVh])
                    if h + 1 < 8:
                        head_prologue(h + 1)
                    cb = catb[h % 2]; Bcb = Bcatb[h % 2]
                    def emit_S(qb):
                        t0, kts = _qb_plan(qb)
                        nw = len(kts)
                        i2 = qb % 2
                        bA, bB = (3, 4) if i2 == 0 else (0, 1)
                        pa_e = pa_es[i2]; Bpae = Bpaes[i2]; pb_e = pb_es[i2]; Bpbe = Bpbes[i2]
                        qs = QT[:, qb * 128:(qb + 1) * 128]
                        for j in range(4):
                            S.op('pe', lambda e: e.matmul(bank(bA)[:, j * 128:(j + 1) * 128], lhsT=KT[:, kts[j] * 128:(kts[j] + 1) * 128], rhs=qs, start=True, stop=True),
                                 reads=[BKT, BQT], writes=[PB[bA]])
                        ktb = ([kts[4]] if nw == 5 else []) + [16, 17]
                        for j, kt in enumerate(ktb):
                            S.op('pe', lambda e: e.matmul(bank(bB)[:, j * 128:(j + 1) * 128], lhsT=KT[:, kt * 128:(kt + 1) * 128], rhs=qs, start=True, stop=True),
                                 reads=[BKT, BQT], writes=[PB[bB]])
                        S.op('act', lambda e: e.activation(out=pa_e[:], in_=bank(bA), func=AF.Exp, scale=scale), reads=[PB[bA]], writes=[Bpae])
                        S.op('dve', lambda e: e.tensor_tensor(out=PA[i2][:], in0=pa_e[:], in1=Eh[:, t0:t0 + 4, :].rearrange("p a b -> p (a b)"), op=ALU.mult),
                             reads=[Bpae, BEh], writes=[BPA[i2]])
                        if nw == 5:
                            S.op('act', lambda e: e.activation(out=pb_e[:], in_=bank(bB, 128), func=AF.Exp, scale=scale), reads=[PB[bB]], writes=[Bpbe])
                            S.op('dve', lambda e: e.tensor_tensor(out=PBt[i2][:, 0:128], in0=pb_e[:], in1=Eh[:, t0 + 4, :], op=ALU.mult),
                                 reads=[Bpbe, BEh], writes=[BPBt[i2]])
                            S.op('act', lambda e: e.activation(out=PBt[i2][:, 128:384], in_=bank(bB)[:, 128:384], func=AF.Exp, scale=scale), reads=[PB[bB]], writes=[BPBt[i2]])
                        else:
                            S.op('act', lambda e: e.activation(out=PBt[i2][:, 0:256], in_=bank(bB)[:, 0:256], func=AF.Exp, scale=scale), reads=[PB[bB]], writes=[BPBt[i2]])
                        return [(PA[i2][:, j * 128:(j + 1) * 128], kts[j], BPA[i2]) for j in range(4)] + \
                               [(PBt[i2][:, j * 128:(j + 1) * 128], kt, BPBt[i2]) for j, kt in enumerate(ktb)]

                    plist_next = emit_S(0)
                    for qb in range(16):
                        qg, ql = qb // 4, qb % 4
                        plist = plist_next
                        if qb + 1 < 16:
                            plist_next = emit_S(qb + 1)
                        bD = 6 + (qg % 2)
                        np_ = len(plist)
                        for j, (pap, kt, Bp) in enumerate(plist):
                            S.op('pe', lambda e: e.matmul(bank(5)[:, ql * 128:(ql + 1) * 128], lhsT=Vh[:, kt, :], rhs=pap, start=(j == 0), stop=(j == np_ - 1)),
                                 reads=[BVh, Bp], writes=[PB[5]])
                        for j, (pap, kt, Bp) in enumerate(plist):
                            S.op('pe', lambda e: e.matmul(bank(bD)[:, ql * 128:(ql + 1) * 128], lhsT=onesb[:], rhs=pap, start=(j == 0), stop=(j == np_ - 1)),
                                 reads=[Bc, Bp], writes=[PB[bD]])
                        if ql == 3:
                            S.op('act', lambda e: e.activation(out=oraw[:], in_=bank(5), func=AF.Copy), reads=[PB[5]], writes=[Boraw])
                            S.op('dve', lambda e: e.reciprocal(out=rden[:], in_=bank(bD)), reads=[PB[bD]], writes=[Brden])
                            S.op('dve', lambda e: e.tensor_tensor(out=osb[:], in0=oraw[:], in1=rden[:], op=ALU.mult), reads=[Boraw, Brden], writes=[Bosb])
                            group_norm_T(osb[:], 512, gv[:, G_GATT + h:G_GATT + h + 1], cb[:, qg * 512:(qg + 1) * 512], Bcb, [Bosb], 2)
                    S.dma('act', cat_d[h], cb[:], reads=[Bcb], writes=[Bcat], owner=Bcb, part=True)
                S.barrier()
            if stop_after == "1b":
                return nc, S

            with ExitStack() as p1c:
                zb = sbt(p1c, "zb", [128, T + 2], F32); bgs = sbt(p1c, "bgs", [128, T], F32)
                cgs = sbt(p1c, "cgs", [128, 512], F32); yb = sbt(p1c, "yb", [128, T], F32)
                Bzb = S.buf("zb"); Bbgs = S.buf("bgs"); Bcgs = S.buf("cgs"); Byb = S.buf("yb")
                S.op('dve', lambda e: e.memset(zb[:, 0:1], 0.0), writes=[Bzb])
                S.op('dve', lambda e: e.memset(zb[:, T + 1:T + 2], 0.0), writes=[Bzb])
                for j in range(8):
                    load_w(0, 3072 + 128 * j); load_w(1, 4096 + 128 * j); load_w(2, 5120 + 128 * j)
                    cast_w(0, 'pool'); cast_w(1, 'pool'); cast_w(2, 'pool')
                    for g in range(4):
                        for slot, pb in ((0, 0), (1, 1), (2, 3)):
                            for k in range(16):
                                S.op('pe', lambda e: e.matmul(bank(pb), lhsT=wb[slot][:, k, :], rhs=hT[:, k, g * 512:(g + 1) * 512],
                                                              start=(k == 0), stop=(k == 15)), reads=[Bwb[slot], BhT], writes=[PB[pb]])
                        S.op('act', lambda e: e.activation(out=bgs[:, g * 512:(g + 1) * 512], in_=bank(0), func=AF.Copy), reads=[PB[0]], writes=[Bbgs])
                        S.op('act', lambda e: e.activation(out=cgs[:], in_=bank(1), func=AF.Copy), reads=[PB[1]], writes=[Bcgs])
                        S.op('dve', lambda e: e.tensor_tensor(out=zb[:, 1 + g * 512:1 + (g + 1) * 512], in0=bank(3), in1=cgs[:], op=ALU.mult),
                             reads=[PB[3], Bcgs], writes=[Bzb])
                    wc = lambda i: gv[:, G_WC + 3 * j + i:G_WC + 3 * j + i + 1]
                    S.op('dve', lambda e: e.tensor_scalar(out=yb[:], in0=zb[:, 1:T + 1], scalar1=wc(1), scalar2=None, op0=ALU.mult), reads=[Bzb, Bc], writes=[Byb])
                    S.op('dve', lambda e: e.scalar_tensor_tensor(out=yb[:], in0=zb[:, 0:T], scalar=wc(0), in1=yb[:], op0=ALU.mult, op1=ALU.add), reads=[Bzb, Bc, Byb], writes=[Byb])
                    S.op('dve', lambda e: e.scalar_tensor_tensor(out=yb[:], in0=zb[:, 2:T + 2], scalar=wc(2), in1=yb[:], op0=ALU.mult, op1=ALU.add), reads=[Bzb, Bc, Byb], writes=[Byb])
                    S.op('pool', lambda e: e.tensor_tensor(out=yb[:], in0=yb[:], in1=bgs[:], op=ALU.mult), reads=[Byb, Bbgs], writes=[Byb])
                    cb = catb[j % 2]; Bcb = Bcatb[j % 2]
                    for g in range(4):
                        group_norm_T(yb[:, g * 512:(g + 1) * 512], 512, gv[:, G_GCONV + j:G_GCONV + j + 1], cb[:, g * 512:(g + 1) * 512], Bcb, [Byb], 2)
                    S.dma('act', cat_d[8 + j], cb[:], reads=[Bcb], writes=[Bcat], owner=Bcb, part=True)
                S.barrier()
        if dbg:
            with ExitStack() as phd:
                tmpc = sbt(phd, "tmpc", [128, T], BF16); Btc = S.buf("tmpc")
                for i in range(16):
                    S.dma('sp', tmpc[:], cat_d[i], reads=[Bcat], writes=[Btc])
                    S.dma('sp', dbg_out["d_cat"][i], tmpc[:], reads=[Btc], writes=[S.buf("dbgc")], owner=Btc)
                S.barrier()
        if stop_after == "1":
            return nc, S

        gt1bc = None
        gt2bc = sbt(es, "gt2bc", [128, D], F32)
        def make_gtbc(ph):
            g1 = sbt(ph, "gt1bc", [128, D], F32)
            gtr = sbt(ph, "gtr", [32, 128], F32)
            Bgtr = S.buf("gtr"); Bgtd = S.buf("gt_d")
            S.op('pe', lambda e: e.transpose(out=bank(1)[:32, 0:128], in_=gt12[:, :], identity=idf[:]), reads=[Bder, Bc], writes=[PB[1]])
            S.op('dve', lambda e: e.tensor_copy(out=gtr[:], in_=bank(1)[:32, 0:128]), reads=[PB[1]], writes=[Bgtr])
            S.dma('sp', gt_d, gtr[:], reads=[Bgtr], writes=[Bgtd])
            for m, dst in ((0, g1), (1, gt2bc)):
                src = gt_d[m * 16:(m + 1) * 16, :].rearrange("k p -> (k p)").partition_broadcast(128)
                S.dma('sp', dst[:], src, reads=[Bgtd], writes=[Bgtbc], part=(m == 1))
            return g1

        Bx1 = S.buf("x1_d"); Bh2 = S.buf("h2_d")
        with ExitStack() as ph:
            gt1bc = make_gtbc(ph)
            wo = sbt(ph, "wo", [128, 16, D], BF16); Bwo = S.buf("wo")
            w_out_v = w_out.rearrange("(k p) f -> p k f", p=128)
            with ExitStack() as p3a:
                wof = [sbt(p3a, f"wof{i}", [128, 16, 128], F32) for i in range(2)]; Bwof = S.bufs("wof", 2)
                for i in range(16):
                    S.dma('sp', wof[i % 2][:], w_out_v[:, :, i * 128:(i + 1) * 128], writes=[Bwof[i % 2]])
                    eng = 'dve' if i % 2 == 0 else 'pool'
                    S.op(eng, lambda e: e.tensor_copy(out=wo[:, :, i * 128:(i + 1) * 128], in_=wof[i % 2][:]), reads=[Bwof[i % 2]], writes=[Bwo])
                S.barrier()
            Bwo.ro = True
            catg = [sbt(ph, f"catg{i}", [128, 16, 512], BF16) for i in range(2)]; Bcatg = S.bufs("catg", 2)
            xt = [sbt(ph, f"xt3{i}", [128, D], F32) for i in range(2)]; Bxt = S.bufs("xt3", 2)
            ytmp = sbt(ph, "ytmp", [128, D], F32); Bytmp = S.buf("ytmp")
            h2g = sbt(ph, "h2g", [128, 16, 512], BF16); Bh2g = S.buf("h2g")
            ntb = make_nt_bufs(ph, "c")

            def load_catg(g):
                S.dma('sp', catg[g % 2][:], cat_d[:, :, g * 512:(g + 1) * 512].rearrange("k p t -> p k t"), reads=[Bcat], writes=[Bcatg[g % 2]])
            load_catg(0)
            S.dma('sp', xt[0][:], x[0:128, :], writes=[Bxt[0]])
            load_tab3, prep_tab3 = make_prep(ph, (2, 3), "p3")
            load_tab3(0)
            ypsum = PS[:, 2048:4096]
            for ti in range(16):
                g = ti // 4
                if ti % 4 == 0 and g + 1 < 4:
                    load_catg(g + 1)
                if ti + 1 < 16:
                    S.dma('sp', xt[(ti + 1) % 2][:], x[(ti + 1) * 128:(ti + 2) * 128, :], writes=[Bxt[(ti + 1) % 2]])
                i2 = ti % 2
                cg_ = catg[g % 2]; Bcg_ = Bcatg[g % 2]
                tl = (ti % 4) * 128
                for k in range(16):
                    for db in range(4):
                        S.op('pe', lambda e: e.matmul(bank(4 + db), lhsT=cg_[:, k, tl:tl + 128], rhs=wo[:, k, db * 512:(db + 1) * 512],
                                                      start=(k == 0), stop=(k == 15)), reads=[Bcg_, Bwo], writes=[PB[4 + db]])
                S.op('dve', lambda e: e.tensor_tensor(out=ytmp[:], in0=ypsum, in1=gt1bc[:], op=ALU.mult),
                     reads=[PB[4], PB[5], PB[6], PB[7], Bgtbc], writes=[Bytmp])
                S.op('pool', lambda e: e.tensor_tensor(out=xt[i2][:], in0=ytmp[:], in1=xt[i2][:], op=ALU.add), reads=[Bytmp, Bxt[i2]], writes=[Bxt[i2]])
                S.dma('act', x1_d[ti * 128:(ti + 1) * 128, :], xt[i2][:], reads=[Bxt[i2]], writes=[Bx1], owner=Bxt[i2], part=True)
                norm_transpose(ntb, xt[i2][:], Bxt[i2], A2, sh2,
                               lambda k: h2g[:, k, tl:tl + 128], Bh2g, (0, 1))
                if ti % 4 == 3:
                    S.dma('act', h2_d[:, :, g * 512:(g + 1) * 512], h2g[:], reads=[Bh2g], writes=[Bh2], owner=Bh2g, part=True)
                for c in range(ti * 4, ti * 4 + 4):
                    prep_tab3(c, 64)
            S.barrier()
        if dbg:
            with ExitStack() as phd:
                tmpx = sbt(phd, "tmpx", [128, D], F32); Btx = S.buf("tmpx")
                for i in range(16):
                    S.dma('sp', tmpx[:], x1_d[i * 128:(i + 1) * 128, :], reads=[Bx1], writes=[Btx])
                    S.dma('sp', dbg_out["d_x1"][i * 128:(i + 1) * 128, :], tmpx[:], reads=[Btx], writes=[S.buf("dbgx")], owner=Btx)
                tmph = sbt(phd, "tmph", [128, 16, 512], BF16); Bth = S.buf("tmph")
                for g in range(4):
                    S.dma('sp', tmph[:], h2_d[:, :, g * 512:(g + 1) * 512], reads=[Bh2], writes=[Bth])
                    S.dma('sp', dbg_out["d_h2"][:, :, g * 512:(g + 1) * 512], tmph[:], reads=[Bth], writes=[S.buf("dbgh2")], owner=Bth)
                S.barrier()
        if stop_after == "3":
            return nc, S

        Bsd = S.buf("s_d")
        with ExitStack() as ph:
            h2T = sbt(ph, "h2T", [128, 16, T], BF16); Bh2T = S.buf("h2T")
            for g in range(4):
                S.dma('sp', h2T[:, :, g * 512:(g + 1) * 512], h2_d[:, :, g * 512:(g + 1) * 512], reads=[Bh2], writes=[Bh2T], part=(g > 0))
            Bh2T.ro = True
            skt = sbt(ph, "skt", [128, 16, 128], F32); Bsk = S.buf("skt")
            S.dma('sp', skt[:], skT, writes=[Bsk])
            Bsk.ro = True
            w_pq_v = w_pq.rearrange("(k p) f -> p k f", p=128)
            wf = [sbt(ph, f"wpf{i}", [128, 16, 128], F32) for i in range(2)]; Bwf = S.bufs("wpf", 2)
            wb = [sbt(ph, f"wpb{i}", [128, 16, 128], BF16) for i in range(2)]; Bwb = S.bufs("wpb", 2)
            qTs = [sbt(ph, f"qTs{i}", [128, 512], F32) for i in range(2)]; BqTs = S.bufs("qTs", 2)
            ssb = [sbt(ph, f"ssb{i}", [128, 512], F32) for i in range(2)]; Bssb = S.bufs("ssb", 2)
            S.dma('sp', wf[0][:], w_pq_v[:, :, 0:128], writes=[Bwf[0]])
            load_tab4, prep_tab4 = make_prep(ph, (4, 5, 6, 7), "p4")
            load_tab4(64)
            cnt = 0
            for j in range(16):
                if j + 1 < 16:
                    S.dma('sp', wf[(j + 1) % 2][:], w_pq_v[:, :, (j + 1) * 128:(j + 2) * 128], writes=[Bwf[(j + 1) % 2]])
                S.op('pool', lambda e: e.tensor_copy(out=wb[j % 2][:], in_=wf[j % 2][:]), reads=[Bwf[j % 2]], writes=[Bwb[j % 2]])
                for g in range(4):
                    i2 = cnt % 2
                    cnt += 1
                    pb = i2
                    for k in range(16):
                        S.op('pe', lambda e: e.matmul(bank(pb), lhsT=wb[j % 2][:, k, :], rhs=h2T[:, k, g * 512:(g + 1) * 512],
                                                      start=(k == 0), stop=(k == 15)), reads=[Bwb[j % 2], Bh2T], writes=[PB[pb]])
                    S.op('act', lambda e: e.activation(out=qTs[i2][:], in_=bank(pb), func=AF.Copy), reads=[PB[pb]], writes=[BqTs[i2]])
                    for t4 in range(4):
                        S.op('pe', lambda e: e.matmul(bank(2 + i2)[:, t4 * 128:(t4 + 1) * 128], lhsT=qTs[i2][:, t4 * 128:(t4 + 1) * 128], rhs=skt[:, j, :],
                                                      start=True, stop=True), reads=[BqTs[i2], Bsk], writes=[PB[2 + i2]])
                    S.op('dve', lambda e: e.tensor_copy(out=ssb[i2][:], in_=bank(2 + i2)), reads=[PB[2 + i2]], writes=[Bssb[i2]])
                    dst = s_d[j, g * 512:(g + 1) * 512, :].rearrange("(a p) k -> p a k", p=128)
                    S.dma('act', dst, ssb[i2][:].rearrange("p (a k) -> p a k", k=128), reads=[Bssb[i2]], writes=[Bsd], owner=Bssb[i2], part=True)
                    prep_tab4(64 + j * 4 + g, 128)
            S.barrier()
        if dbg:
            with ExitStack() as phd:
                tmps = sbt(phd, "tmps", [128, 16, 128], F32); Bts = S.buf("tmps")
                for i in range(16):
                    S.dma('sp', tmps[:], s_d[:, i * 128:(i + 1) * 128, :].rearrange("j p k -> p j k"), reads=[Bsd], writes=[Bts])
                    S.dma('sp', dbg_out["d_s"][i * 128:(i + 1) * 128, :].rearrange("p (j k) -> p j k", k=128), tmps[:], reads=[Bts], writes=[S.buf("dbgs")], owner=Bts)
                S.barrier()
        if stop_after == "4a":
            return nc, S

        BGT = S.buf("GT_d")
        NSUB = 16
        with ExitStack() as ph:
            stm = [sbt(ph, "stm0", [128, 16, 128], F32)] * 2; Bstm = [S.buf("stm0")] * 2
            wk = sbt(ph, "wk", [128, 16, 128], F32); Bwk = S.buf("wk")
            top = sbt(ph, "top", [128, 16, 16], F32); Btop = S.buf("top")
            cand = sbt(ph, "cand", [128, 8, 16, 16], F32); Bcand = S.buf("cand")
            cwk = wk[:].rearrange("p (h two) k -> p h (two k)", two=2); Bcwk = Bwk
            best = sbt(ph, "best", [128, 8, 16], F32); Bbest = S.buf("best")
            eb = sbt(ph, "eb", [128, 8, 16], F32); Beb = S.buf("eb")
            Zs = sbt(ph, "Zs", [128, 8], F32); rZ = sbt(ph, "rZ", [128, 8], F32); BZ = S.buf("Z")
            tm4 = sbt(ph, "tm4", [128, 4, 8, 16], F32); Btm4 = S.buf("tm4")
            colT = [sbt(ph, f"colT{i}", [128, 4, 128], F32) for i in range(2)]; BcolT = S.bufs("colT", 2)
            S0 = [sbt(ph, f"S0r{i}", [128, NSUB, 128], F32) for i in range(2)]; BS0 = S.bufs("S0r", 2)
            S1 = [sbt(ph, f"S1r{i}", [128, NSUB, 128], F32) for i in range(2)]; BS1 = S.bufs("S1r", 2)
            Mks = [sbt(ph, f"Mk{i}", [128, NSUB, 128], BF16) for i in range(2)]; BMks = S.bufs("Mk", 2)
            D1s = [sbt(ph, f"D1{i}", [128, NSUB, 128], F32) for i in range(2)]; BD1s = S.bufs("D1", 2)
            E1s = [sbt(ph, f"E1{i}", [128, NSUB, 128], BF16) for i in range(2)]; BE1s = S.bufs("E1", 2)
            Rr = [sbt(ph, f"Rr{i}", [128, NSUB, 128], BF16) for i in range(2)]; BRr = S.bufs("Rr", 2)
            OH = [sbt(ph, f"OH{i}", [128, NSUB, 128], BF16) for i in range(2)]; BOH = S.bufs("OH", 2)
            GTs = [sbt(ph, "GTs0", [128, 128, 128], BF16)] * 2; BGTs = [S.buf("GTs0")] * 2
            nsub = 128 // NSUB
            def load_rep(ti, sb_i, slot):
                tok0 = ti * 128 + sb_i * NSUB
                for p, (dst, Bd) in enumerate(((S0[slot], BS0[slot]), (S1[slot], BS1[slot]))):
                    src = bass.AP(s_d.tensor, p * T * 128 + tok0 * 128, [[2 * T * 128, 8], [0, 16], [1, NSUB * 128]])
                    S.dma('sp', dst[:].rearrange("q t k -> q (t k)"), src, reads=[Bsd], writes=[Bd])

            S.dma('sp', stm[0][:], s_d[:, 0:128, :].rearrange("j p k -> p j k"), reads=[Bsd], writes=[Bstm[0]])
            gsub = 0
            for ti in range(16):
                st = stm[ti % 2]; Bst_ = Bstm[ti % 2]
                load_rep(ti, 0, 0)
                for j in range(16):
                    S.op('dve', lambda e: e.max(out=top[:, j, 0:8], in_=st[:, j, :]), reads=[Bst_], writes=[Btop])
                    S.op('dve', lambda e: e.match_replace(out=wk[:, j, :], in_to_replace=top[:, j, 0:8], in_values=st[:, j, :], imm_value=NEG),
                         reads=[Bst_, Btop], writes=[Bwk])
                    S.op('dve', lambda e: e.max(out=top[:, j, 8:16], in_=wk[:, j, :]), reads=[Bwk], writes=[Btop])
                if ti + 1 < 16:
                    S.dma('sp', stm[(ti + 1) % 2][:], s_d[:, (ti + 1) * 128:(ti + 2) * 128, :].rearrange("j p k -> p j k"), reads=[Bsd], writes=[Bstm[(ti + 1) % 2]])
                tv = top[:].rearrange("p (h two) a -> p h two a", two=2)
                S.op('dve', lambda e: e.tensor_tensor(out=cand[:], in0=tv[:, :, 0, :].unsqueeze(3).to_broadcast([128, 8, 16, 16]),
                                                      in1=tv[:, :, 1, :].unsqueeze(2).to_broadcast([128, 8, 16, 16]), op=ALU.add), reads=[Btop], writes=[Bcand])
                cv = cand[:].rearrange("p h a b -> p h (a b)")
                for h in range(8):
                    S.op('dve', lambda e: e.max(out=best[:, h, 0:8], in_=cv[:, h, :]), reads=[Bcand], writes=[Bbest])
                    S.op('dve', lambda e: e.match_replace(out=cwk[:, h, :], in_to_replace=best[:, h, 0:8], in_values=cv[:, h, :], imm_value=NEG),
                         reads=[Bcand, Bbest], writes=[Bcwk])
                    S.op('dve', lambda e: e.max(out=best[:, h, 8:16], in_=cwk[:, h, :]), reads=[Bcwk], writes=[Bbest])
                S.op('dve', lambda e: e.tensor_tensor(out=eb[:], in0=best[:], in1=best[:, :, 0:1].to_broadcast([128, 8, 16]), op=ALU.subtract), reads=[Bbest], writes=[Beb])
                S.op('act', lambda e: e.activation(out=eb[:], in_=eb[:], func=AF.Exp), reads=[Beb], writes=[Beb])
                S.op('dve', lambda e: e.tensor_reduce(out=Zs[:], in_=eb[:], axis=AX.X, op=ALU.add), reads=[Beb], writes=[BZ])
                S.op('dve', lambda e: e.tensor_copy(out=tm4[:, 0, :, :], in_=tv[:, :, 0, :]), reads=[Btop], writes=[Btm4])
                S.op('dve', lambda e: e.tensor_copy(out=tm4[:, 2, :, :], in_=best[:, :, 15:16].to_broadcast([128, 8, 16])), reads=[Bbest], writes=[Btm4])
                S.op('act', lambda e: e.activation(out=rZ[:], in_=Zs[:], func=AF.Ln), reads=[BZ], writes=[BZ])
                S.op('dve', lambda e: e.tensor_tensor(out=tm4[:, 3, :, :], in0=tv[:, :, 0, :], in1=best[:, :, 0:1].to_broadcast([128, 8, 16]), op=ALU.subtract),
                     reads=[Btop, Bbest], writes=[Btm4])
                S.op('dve', lambda e: e.tensor_tensor(out=tm4[:, 3, :, :], in0=tm4[:, 3, :, :], in1=rZ[:].unsqueeze(2).to_broadcast([128, 8, 16]), op=ALU.subtract),
                     reads=[Btm4, BZ], writes=[Btm4])
                cT = colT[ti % 2]; BcT = BcolT[ti % 2]
                for q in (0, 2, 3):
                    S.op('pe', lambda e: e.transpose(out=bank(7)[:, q * 128:(q + 1) * 128], in_=tm4[:, q, :, :].rearrange("p h a -> p (h a)"), identity=idf[:]),
                         reads=[Btm4, Bc], writes=[PB[7]])
                S.op('act', lambda e: e.activation(out=cT[:].rearrange("p q t -> p (q t)"), in_=bank(7), func=AF.Copy), reads=[PB[7]], writes=[BcT])
                GTt = GTs[ti % 2]; BGTt = BGTs[ti % 2]
                def stageA(sb_i):
                    slot = sb_i % 2
                    tl = sb_i * NSUB

                    def bc(qi):
                        return cT[:, qi, tl:tl + NSUB].unsqueeze(2).to_broadcast([128, NSUB, 128])
                    s0r = S0[slot]; s1r = S1[slot]
                    D1 = D1s[slot]; BD1 = BD1s[slot]; E1 = E1s[slot]; BE1 = BE1s[slot]; Mk = Mks[slot]; BMk = BMks[slot]
                    S.op('pool', lambda e: e.tensor_tensor(out=D1[:], in0=s1r[:], in1=bc(3), op=ALU.add), reads=[BS1[slot], BcT], writes=[BD1])
                    S.op('act', lambda e: e.activation(out=E1[:], in_=D1[:], func=AF.Exp), reads=[BD1], writes=[BE1])
                    S.op('dve', lambda e: e.tensor_tensor(out=s1r[:], in0=s1r[:], in1=bc(0), op=ALU.add), reads=[BS1[slot], BcT], writes=[BS1[slot]])
                    S.op('dve', lambda e: e.tensor_tensor(out=Mk[:], in0=s1r[:], in1=bc(2), op=ALU.is_ge), reads=[BS1[slot], BcT], writes=[BMk])
                    S.op('dve', lambda e: e.tensor_tensor(out=OH[slot][:], in0=s0r[:], in1=bc(0), op=ALU.is_equal), reads=[BS0[slot], BcT], writes=[BOH[slot]])
                    S.op('pool', lambda e: e.tensor_tensor(out=Rr[slot][:], in0=Mk[:], in1=E1[:], op=ALU.mult), reads=[BMk, BE1], writes=[BRr[slot]])

                def stageB(sb_i):
                    slot = sb_i % 2
                    tl = sb_i * NSUB
                    for t4 in range(NSUB // 4):
                        pb = 5 + (t4 % 2)
                        for tt in range(4):
                            t = t4 * 4 + tt
                            S.op('pe', lambda e: e.matmul(bank(pb)[:, tt * 128:(tt + 1) * 128], lhsT=Rr[slot][:, t, :], rhs=OH[slot][:, t, :], start=True, stop=True),
                                 reads=[BRr[slot], BOH[slot]], writes=[PB[pb]])
                        dst = GTt[:, :, tl + t4 * 4:tl + t4 * 4 + 4]
                        src = bank(pb).rearrange("p (t c) -> p c t", c=128)
                        S.op('act', lambda e: e.activation(out=dst, in_=src, func=AF.Copy), reads=[PB[pb]], writes=[BGTt])

                load_rep(ti, 1, 1)
                stageA(0)
                for sb_i in range(nsub):
                    if sb_i + 2 < nsub:
                        load_rep(ti, sb_i + 2, sb_i % 2)
                    if sb_i + 1 < nsub:
                        stageA(sb_i + 1)
                    stageB(sb_i)
                for c8 in range(8):
                    dst = GT_d[c8 * 16:(c8 + 1) * 16, :, ti * 128:(ti + 1) * 128].rearrange("c p t -> p c t")
                    S.dma('act', dst, GTt[:, c8 * 16:(c8 + 1) * 16, :], reads=[BGTt], writes=[BGT], owner=BGTt, part=True, maxfly=2)
            S.barrier()
        if dbg:
            with ExitStack() as phd:
                tmpg = sbt(phd, "tmpg", [128, 8, T], BF16); Btg = S.buf("tmpg")
                for i in range(16):
                    S.dma('sp', tmpg[:], GT_d[i * 8:(i + 1) * 8].rearrange("c p t -> p c t"), reads=[BGT], writes=[Btg])
                    S.dma('sp', dbg_out["d_GT"][i * 8:(i + 1) * 8].rearrange("c p t -> p c t"), tmpg[:], reads=[Btg], writes=[S.buf("dbgg")], owner=Btg)
                S.barrier()
        if stop_after == "4b":
            return nc, S

        Bout = S.buf("out")
        SC = 4
        NSC = 128 // SC
        with ExitStack() as ph:
            h2g = sbt(ph, "h2g50", [128, 16, 512], BF16); Bhg = S.buf("h2g50")
            acc = sbt(ph, "acc", [128, 4, D], F32); Bacc = S.buf("acc")
            uTc = [sbt(ph, f"uTc{i}", [128, D], BF16) for i in range(3)]; BuTc = S.bufs("uTc", 3)
            vcs = [sbt(ph, f"vcs{i}", [128, SC, D], BF16) for i in range(2)]; Bvcs = S.bufs("vcs", 2)
            gtc = [sbt(ph, f"gtc{i}", [128, 512], BF16) for i in range(3)]; Bgtc = S.bufs("gtc", 3)
            gl = [sbt(ph, f"gl{i}", [128, 512], BF16) for i in range(2)]; Bgl = S.bufs("gl", 2)
            AG = [sbt(ph, f"AG{i}", [128, SC, 512], BF16) for i in range(2)]; BAG = S.bufs("AG", 2)
            x1t = sbt(ph, "x1f0", [128, D], F32); Bx1t = S.buf("x1f0")
            ot = [sbt(ph, f"ot{i}", [128, D], F32) for i in range(2)]; Bot = S.bufs("ot", 2)

            vfs = [sbt(ph, f"vfs{i}", [128, D], F32) for i in range(4)]; Bvfs = S.bufs("vfs", 4)

            def load_chunk(g, c):
                i3 = c % 3
                S.dma('sp', uTc[i3][:], uT_d[c], reads=[BuT], writes=[BuTc[i3]])
                S.dma('sp', gtc[i3][:], GT_d[c, :, g * 512:(g + 1) * 512], reads=[BGT], writes=[Bgtc[i3]])
                S.dma('sp', vfs[c % 4][:], v_tab[c * 128:(c + 1) * 128, :], writes=[Bvfs[c % 4]])

            def load_v(sc):
                pass

            def first_phase(g, sc):
                ag = AG[sc % 2]; Bag = BAG[sc % 2]
                for cl in range(SC):
                    c = sc * SC + cl
                    if c + 2 < 128:
                        load_chunk(g, c + 2)
                    i3 = c % 3
                    pb = c % 2
                    for k in range(16):
                        S.op('pe', lambda e: e.matmul(bank(pb), lhsT=uTc[i3][:, k * 128:(k + 1) * 128], rhs=h2g[:, k, :], start=(k == 0), stop=(k == 15)),
                             reads=[BuTc[i3], Bhg], writes=[PB[pb]])
                    S.op('act', lambda e: e.activation(out=gl[pb][:], in_=bank(pb), func=AF.Gelu), reads=[PB[pb]], writes=[Bgl[pb]])
                    S.op('pool', lambda e: e.tensor_tensor(out=ag[:, cl, :], in0=gl[pb][:], in1=gtc[i3][:], op=ALU.mult), reads=[Bgl[pb], Bgtc[i3]], writes=[Bag])
                    S.op('act', lambda e: e.activation(out=vcs[sc % 2][:, cl, :], in_=vfs[c % 4][:], func=AF.Copy), reads=[Bvfs[c % 4]], writes=[Bvcs[sc % 2]])

            def second_phase(sc):
                ag = AG[sc % 2]; Bag = BAG[sc % 2]
                vv = vcs[sc % 2]; Bvv = Bvcs[sc % 2]
                for tt in range(4):
                    for db in range(4):
                        pb = 2 + (tt * 4 + db) % 6
                        for cl in range(SC):
                            S.op('pe', lambda e: e.matmul(bank(pb), lhsT=ag[:, cl, tt * 128:(tt + 1) * 128], rhs=vv[:, cl, db * 512:(db + 1) * 512],
                                                          start=(cl == 0), stop=(cl == SC - 1)), reads=[Bag, Bvv], writes=[PB[pb]])
                        a_ap = acc[:, tt, db * 512:(db + 1) * 512]
                        if sc == 0:
                            S.op('dve', lambda e: e.tensor_copy(out=a_ap, in_=bank(pb)), reads=[PB[pb]], writes=[Bacc])
                        else:
                            S.op('dve', lambda e: e.tensor_tensor(out=a_ap, in0=bank(pb), in1=a_ap, op=ALU.add), reads=[PB[pb], Bacc], writes=[Bacc])

            for g in range(4):
                S.dma('sp', h2g[:], h2_d[:, :, g * 512:(g + 1) * 512], reads=[Bh2], writes=[Bhg])
                load_chunk(g, 0)
                load_chunk(g, 1)
                load_v(0)
                for sc in range(NSC):
                    first_phase(g, sc)
                    if sc > 0:
                        second_phase(sc - 1)
                    if sc + 1 < NSC:
                        load_v(sc + 1)
                second_phase(NSC - 1)
                for tt in range(4):
                    ti = g * 4 + tt
                    i2 = ti % 2
                    S.dma('sp', x1t[:], x1_d[ti * 128:(ti + 1) * 128, :], reads=[Bx1], writes=[Bx1t])
                    S.op('dve', lambda e: e.tensor_tensor(out=ot[i2][:], in0=acc[:, tt, :], in1=gt2bc[:], op=ALU.mult), reads=[Bacc, Bgtbc], writes=[Bot[i2]])
                    S.op('pool', lambda e: e.tensor_tensor(out=ot[i2][:], in0=ot[i2][:], in1=x1t[:], op=ALU.add), reads=[Bot[i2], Bx1t], writes=[Bot[i2]])
                    S.dma('act', out[ti * 128:(ti + 1) * 128, :], ot[i2][:], reads=[Bot[i2]], writes=[Bout], owner=Bot[i2], part=True)
            S.barrier()
    return nc, S


_CACHE = {}


def kernel(**inputs):
    inp = {k: np.asarray(v) for k, v in inputs.items()}
    if "nc" not in _CACHE:
        _CACHE["nc"] = build_program()[0]
    nc = _CACHE["nc"]
    in_maps = [_host_inputs(inp, b) for b in range(8)]
    res = run_bass_kernel_spmd(nc, in_maps, core_ids=list(range(8)))
    return np.stack([np.asarray(r["out"], np.float32) for r in res.results], axis=0)
```
